# Optimizing a Trainium2 kernel written in Bass

```python
import jax, jax.numpy as jnp
from jax import lax
import numpy as np

D_MODEL = 4096
BATCH = 2
SEQ = 8192
DEPTH = 4

N_MIXERS = 2
N_POOL_LAYERS = (DEPTH + 1) // 2
N_GMLP_LAYERS = DEPTH // 2
POOL_WINDOWS = (2, 4, 8, 16)
N_POOL_GROUPS = len(POOL_WINDOWS)
POOL_GROUP_W = D_MODEL // N_POOL_GROUPS
GMLP_WIDTH = D_MODEL
GMLP_HEADS = 8
GMLP_HEAD_W = GMLP_WIDTH // GMLP_HEADS
GMLP_CHUNK = 128
N_EXPERTS = 32
TOP_K = 4
D_EXPERT = 384
SWIGLU_LIMIT = 7.0
SWIGLU_ALPHA = 1.702
COND_RANK = 512
N_MOD = 6
MOE_BLOCK = 128
NORM_EPS = 1e-5

kernel_name = 'hybrid_pool_gmlp_moe_adaln'


def _rms_norm(x, g):
    xf = x.astype(jnp.float32)
    y = xf * lax.rsqrt(jnp.mean(xf * xf, axis=-1, keepdims=True) + NORM_EPS)
    return (y * g.astype(jnp.float32)).astype(x.dtype)


def _layer_norm(x, g, b):
    xf = x.astype(jnp.float32)
    mu = jnp.mean(xf, axis=-1, keepdims=True)
    xc = xf - mu
    y = xc * lax.rsqrt(jnp.mean(xc * xc, axis=-1, keepdims=True) + NORM_EPS)
    return (y * g.astype(jnp.float32) + b.astype(jnp.float32)).astype(x.dtype)


def _pool_mixer(h, w_in, w_group, scale, w_out):
    bsz, s, _ = h.shape
    u = (h @ w_in).reshape(bsz, s, N_POOL_GROUPS, POOL_GROUP_W)
    uf = u.astype(jnp.float32)
    cs = jnp.cumsum(uf, axis=1)
    pos = jnp.arange(s)
    outs = []
    for g, w in enumerate(POOL_WINDOWS):
        csg = cs[:, :, g]
        lagged = jnp.pad(csg, ((0, 0), (w, 0), (0, 0)))[:, :s]
        cnt = jnp.minimum(pos + 1, w).astype(jnp.float32)[None, :, None]
        outs.append((csg - lagged) / cnt - uf[:, :, g])
    pooled = jnp.stack(outs, axis=2).astype(h.dtype)
    mixed = jnp.einsum('bsgc,gcd->bsgd', pooled, w_group).reshape(bsz, s, D_MODEL)
    return (mixed * scale) @ w_out


def _gmlp_mixer(h, w_in, b_in, ln_g, ln_b, w_s, b_s, w_out):
    bsz, s, _ = h.shape
    uv = jax.nn.gelu(h @ w_in + b_in, approximate=False)
    u, v = uv[..., :GMLP_WIDTH], uv[..., GMLP_WIDTH:]
    v = _layer_norm(v, ln_g, ln_b)
    n_chunks = s // GMLP_CHUNK
    vc = v.reshape(bsz, n_chunks, GMLP_CHUNK, GMLP_HEADS, GMLP_HEAD_W)
    causal = jnp.tril(jnp.ones((GMLP_CHUNK, GMLP_CHUNK), dtype=bool))
    w_causal = jnp.where(causal[None], w_s, jnp.zeros_like(w_s))
    mixed = jnp.einsum('hts,bnshc->bnthc', w_causal, vc)
    mixed = mixed + jnp.swapaxes(b_s, 0, 1)[None, None, :, :, None]
    return (u * mixed.reshape(bsz, s, GMLP_WIDTH)) @ w_out


def _clamped_swiglu(hu):
    glu, lin = hu[..., :D_EXPERT], hu[..., D_EXPERT:]
    glu = jnp.minimum(glu, SWIGLU_LIMIT)
    lin = jnp.clip(lin, -SWIGLU_LIMIT, SWIGLU_LIMIT)
    return glu * jax.nn.sigmoid(SWIGLU_ALPHA * glu) * (lin + 1.0)


def _moe(h, w_router, b_router, w_up, b_up, w_down, b_down):
    bsz, s, d = h.shape
    n_tok = bsz * s
    xt = h.reshape(n_tok, d)
    logits = xt.astype(jnp.float32) @ w_router.astype(jnp.float32) + b_router.astype(jnp.float32)
    top_vals, top_idx = lax.top_k(logits, TOP_K)
    gates = jax.nn.softmax(top_vals, axis=-1)
    e_flat = top_idx.reshape(-1).astype(jnp.int32)
    tok_flat = jnp.repeat(jnp.arange(n_tok, dtype=jnp.int32), TOP_K)
    g_flat = gates.reshape(-1)
    order = jnp.argsort(e_flat)
    e_sorted = e_flat[order]
    counts = jnp.bincount(e_flat, length=N_EXPERTS)
    padded = (counts + MOE_BLOCK - 1) // MOE_BLOCK * MOE_BLOCK
    start = jnp.cumsum(counts) - counts
    pend = jnp.cumsum(padded)
    pstart = pend - padded
    n_pairs = n_tok * TOP_K
    dest = pstart[e_sorted] + (jnp.arange(n_pairs, dtype=jnp.int32) - start[e_sorted])
    n_rows = n_pairs + N_EXPERTS * MOE_BLOCK
    n_blocks = n_rows // MOE_BLOCK
    row_tok = jnp.zeros((n_rows,), jnp.int32).at[dest].set(tok_flat[order])
    row_gate = jnp.zeros((n_rows,), jnp.float32).at[dest].set(g_flat[order])
    blk_start = jnp.arange(n_blocks, dtype=jnp.int32) * MOE_BLOCK
    blk_expert = jnp.minimum(jnp.searchsorted(pend, blk_start, side='right'), N_EXPERTS - 1)

    def block_fn(args):
        e, toks = args
        xb = xt[toks]
        act = _clamped_swiglu(xb @ w_up[e] + b_up[e])
        return act @ w_down[e] + b_down[e]

    yb = lax.map(block_fn, (blk_expert, row_tok.reshape(n_blocks, MOE_BLOCK)))
    yb = yb.reshape(n_rows, d) * row_gate[:, None].astype(yb.dtype)
    out = jax.ops.segment_sum(yb, row_tok, num_segments=n_tok)
    return out.reshape(bsz, s, d)


def setup_inputs(seed: int = 0) -> dict:
    key = jax.random.key(seed)
    ks = jax.random.split(key, 28)
    f32 = jnp.float32

    def nrm(k, shape, scale):
        return jax.random.normal(k, shape, f32) * scale

    D, E2 = D_MODEL, 2 * GMLP_WIDTH
    return {
        'x': nrm(ks[0], (BATCH, SEQ, D), 1.0),
        'c': nrm(ks[1], (BATCH, D), 1.0),
        'w_cond': nrm(ks[2], (D, COND_RANK), D ** -0.5),
        'b_cond': nrm(ks[3], (COND_RANK,), 0.02),
        'w_mod': nrm(ks[4], (DEPTH, COND_RANK, N_MOD * D), 0.3 * COND_RANK ** -0.5),
        'b_mod': nrm(ks[5], (DEPTH, N_MOD * D), 0.02),
        'g_norm_mix': 1.0 + nrm(ks[6], (DEPTH, D), 0.02),
        'g_norm_ffn': 1.0 + nrm(ks[7], (DEPTH, D), 0.02),
        'pool_w_in': nrm(ks[8], (N_POOL_LAYERS, D, D), D ** -0.5),
        'pool_w_group': nrm(ks[9], (N_POOL_LAYERS, N_POOL_GROUPS, POOL_GROUP_W, POOL_GROUP_W), POOL_GROUP_W ** -0.5),
        'pool_scale': 1.0 + nrm(ks[10], (N_POOL_LAYERS, D), 0.1),
        'pool_w_out': nrm(ks[11], (N_POOL_LAYERS, D, D), D ** -0.5),
        'gmlp_w_in': nrm(ks[12], (N_GMLP_LAYERS, D, E2), D ** -0.5),
        'gmlp_b_in': nrm(ks[13], (N_GMLP_LAYERS, E2), 0.02),
        'gmlp_ln_g': 1.0 + nrm(ks[14], (N_GMLP_LAYERS, GMLP_WIDTH), 0.02),
        'gmlp_ln_b': nrm(ks[15], (N_GMLP_LAYERS, GMLP_WIDTH), 0.02),
        'gmlp_w_s': nrm(ks[16], (N_GMLP_LAYERS, GMLP_HEADS, GMLP_CHUNK, GMLP_CHUNK), 0.5 * GMLP_CHUNK ** -0.5),
        'gmlp_b_s': 1.0 + nrm(ks[17], (N_GMLP_LAYERS, GMLP_HEADS, GMLP_CHUNK), 0.1),
        'gmlp_w_out': nrm(ks[18], (N_GMLP_LAYERS, GMLP_WIDTH, D), GMLP_WIDTH ** -0.5),
        'moe_w_router': nrm(ks[19], (DEPTH, D, N_EXPERTS), D ** -0.5),
        'moe_b_router': nrm(ks[20], (DEPTH, N_EXPERTS), 0.01),
        'moe_w_up': nrm(ks[21], (DEPTH, N_EXPERTS, D, 2 * D_EXPERT), D ** -0.5),
        'moe_b_up': nrm(ks[22], (DEPTH, N_EXPERTS, 2 * D_EXPERT), 0.02),
        'moe_w_down': nrm(ks[23], (DEPTH, N_EXPERTS, D_EXPERT, D), D_EXPERT ** -0.5),
        'moe_b_down': nrm(ks[24], (DEPTH, N_EXPERTS, D), 0.02),
        'g_final': 1.0 + nrm(ks[25], (D,), 0.02),
    }


def reference(x, c, w_cond, b_cond, w_mod, b_mod, g_norm_mix, g_norm_ffn,
              pool_w_in, pool_w_group, pool_scale, pool_w_out,
              gmlp_w_in, gmlp_b_in, gmlp_ln_g, gmlp_ln_b, gmlp_w_s, gmlp_b_s, gmlp_w_out,
              moe_w_router, moe_b_router, moe_w_up, moe_b_up, moe_w_down, moe_b_down,
              g_final):
    cond = jax.nn.silu(c @ w_cond + b_cond)
    for layer in range(DEPTH):
        mod = (cond @ w_mod[layer] + b_mod[layer])[:, None, :]
        sh1, sc1, gt1, sh2, sc2, gt2 = jnp.split(mod, N_MOD, axis=-1)
        j = layer // N_MIXERS
        h = _rms_norm(x, g_norm_mix[layer]) * (1.0 + sc1) + sh1
        if layer % N_MIXERS == 0:
            y = _pool_mixer(h, pool_w_in[j], pool_w_group[j], pool_scale[j], pool_w_out[j])
        else:
            y = _gmlp_mixer(h, gmlp_w_in[j], gmlp_b_in[j], gmlp_ln_g[j], gmlp_ln_b[j],
                            gmlp_w_s[j], gmlp_b_s[j], gmlp_w_out[j])
        x = x + gt1 * y
        h = _rms_norm(x, g_norm_ffn[layer]) * (1.0 + sc2) + sh2
        x = x + gt2 * _moe(h, moe_w_router[layer], moe_b_router[layer], moe_w_up[layer],
                           moe_b_up[layer], moe_w_down[layer], moe_b_down[layer])
    return _rms_norm(x, g_final)
```

```python
import contextlib
import numpy as np
import concourse.bass as bass
import concourse.mybir as mybir
from concourse.bass_utils import run_bass_kernel_spmd

F32 = mybir.dt.float32
BF16 = mybir.dt.bfloat16
I32 = mybir.dt.int32
AF = mybir.ActivationFunctionType
ALU = mybir.AluOpType
AX = mybir.AxisListType
DSIZE = {F32: 4, BF16: 2, I32: 4}


class Cfg:
    def __init__(self, D=4096, SEQ=8192, BATCH=2, DEPTH=4, HEADS=8, FE=384, E=32, RANK=512,
                 BS=2, HALO=256, NQ=2, ARENA_KB=206):
        self.D, self.SEQ, self.BATCH, self.DEPTH, self.HEADS = D, SEQ, BATCH, DEPTH, HEADS
        self.BS = BS
        self.BLK = BS * 128
        self.FE, self.E, self.RANK, self.HALO, self.NQ = FE, E, RANK, HALO, NQ
        self.DC = D // 128
        self.RC = RANK // 128
        self.SQ = SEQ // NQ
        self.TC = self.SQ + HALO
        self.NT = self.TC // 128
        self.GW = D // 4
        self.HW = D // HEADS
        self.FC = FE // 128
        self.NB = (self.TC * 4 + self.BLK - 1) // self.BLK + E
        self.NSLOT = self.NB * self.BLK
        self.ARENA_KB = ARENA_KB
        self.NBW = min(512, D)
        tt = []
        t0 = 0
        while t0 < self.TC:
            tw = min(512, self.TC - t0)
            tt.append((t0, tw))
            t0 += tw
        self.ttiles = tt


class Op:
    __slots__ = ("eng", "fn", "deps", "inc", "ticket", "dma", "rw")

    def __init__(self, eng, fn, dma):
        self.eng, self.fn, self.dma = eng, fn, dma
        self.deps = []
        self.inc = dma is not None
        self.ticket = None


class Tile:
    def __init__(self, ap, dkey=None):
        self.ap = ap
        self.dkey = dkey

    def __getitem__(self, k):
        return self.ap[k]


class Prog:
    ENGS = ("pe", "act", "dve", "pool", "sp")
    BLK = {"pe": "tensor", "act": "scalar", "dve": "vector", "pool": "gpsimd", "sp": "sync"}
    LIMIT = 20000

    def __init__(self, nc, stack, arena_kb, dry=False):
        self.nc, self.stack = nc, stack
        self.ops = []
        self.lastw = {}
        self.readers = {}
        self.epoch = []
        self.epoch_done = {e: True for e in self.ENGS}
        self.last_inc = {e: None for e in self.ENGS}
        self.dma_last = {}
        self.regs = {}
        self.arena_elems = arena_kb * 1024 // 2
        if dry:
            self.arena_elems = 1024 * 512
            self.arena = nc.dram_tensor("arena_dry", [128, self.arena_elems], BF16).ap()
        else:
            self.arena = stack.enter_context(nc.sbuf_tensor("arena", [128, self.arena_elems], BF16))
        self.pbase = 0
        self.sp_off = 0
        self.psum = [Tile(stack.enter_context(nc.psum_tensor("ps%d" % i, [128, 512], F32))[:]) for i in range(8)]

    def _carve(self, off_bytes, shape, dtype):
        per = int(np.prod(shape[1:])) * DSIZE[dtype]
        a = self.arena[0:shape[0], off_bytes // 2:(off_bytes + per) // 2]
        if dtype != BF16:
            a = a.bitcast(dtype)
        if len(shape) == 3:
            a = a.rearrange("p (a b) -> p a b", a=shape[1])
        elif len(shape) == 4:
            a = a.rearrange("p (a b c) -> p a b c", a=shape[1], b=shape[2])
        return a, per

    def persist(self, shape, dtype, dkey=None):
        a, per = self._carve(self.pbase, shape, dtype)
        self.pbase += (per + 63) // 64 * 64
        self.sp_off = self.pbase
        return Tile(a, dkey)

    def tile(self, shape, dtype, dkey=None):
        per = int(np.prod(shape[1:])) * DSIZE[dtype]
        self.hwm = max(getattr(self, "hwm", 0), self.sp_off + per)
        if self.sp_off + per > self.arena_elems * 2:
            raise RuntimeError("arena overflow: need %d > %d" % (self.sp_off + per, self.arena_elems * 2))
        a, per = self._carve(self.sp_off, shape, dtype)
        self.sp_off += (per + 63) // 64 * 64
        return Tile(a, dkey)

    def stage(self, keep=None):
        self.epoch = [op for op in self.last_inc.values() if op is not None] + list(self.dma_last.values())
        for op in self.epoch:
            op.inc = True
        self.epoch_done = {e: False for e in self.ENGS}
        self.sp_off = self.pbase if keep is None else keep
        self.lastw.clear()
        self.readers.clear()

    def add(self, eng, fn, reads=(), writes=(), dma=None):
        if dma is not None:
            dma = "%s_%s" % (eng, dma)
        op = Op(eng, fn, dma)
        op.rw = (tuple(reads), tuple(writes))
        deps = {}

        def need(d, raw):
            if d is None or d is op:
                return
            if d.dma is None and dma is None and d.eng == eng and eng == "pe":
                return
            deps[id(d)] = d

        for r in reads:
            need(self.lastw.get(r), True)
        for w in writes:
            need(self.lastw.get(w), False)
            for rd in self.readers.get(w, ()):
                need(rd, False)
        if dma is not None:
            need(self.dma_last.get(dma), False)
            self.dma_last[dma] = op
        if not self.epoch_done[eng]:
            for d in self.epoch:
                if d is not op:
                    deps[id(d)] = d
            self.epoch_done[eng] = True
        for d in deps.values():
            d.inc = True
        op.deps = list(deps.values())
        for w in writes:
            self.lastw[w] = op
            self.readers[w] = []
        for r in reads:
            self.readers.setdefault(r, []).append(op)
        if dma is None:
            self.last_inc[eng] = op
        self.ops.append(op)
        return op

    def reg(self, e, val):
        if val not in self.regs:
            self.regs[val] = e.to_reg(val)
        return self.regs[val]

    def dma(self, q, out, in_, reads=(), writes=(), key=None, **kw):
        assert key is not None
        return self.add(q, lambda e: e.dma_start(out=out, in_=in_, **kw), reads, writes, dma=key)

    def emit(self):
        nc, stack = self.nc, self.stack
        self.stage()
        fin = self.epoch
        eng_sems = {e: [] for e in self.ENGS}
        cnt = {e: 0 for e in self.ENGS}
        dsem = {}
        nsem = [0]

        def newsem(name):
            nsem[0] += 1
            return stack.enter_context(nc.semaphore(name))

        for op in self.ops:
            if op.dma is not None:
                if op.dma not in dsem:
                    dsem[op.dma] = [newsem("d_%s" % op.dma), 0]
                s = dsem[op.dma]
                s[1] += 16
                op.ticket = (s[0], s[1])
            elif op.inc:
                if not eng_sems[op.eng] or cnt[op.eng] >= self.LIMIT:
                    eng_sems[op.eng].append(newsem("e_%s_%d" % (op.eng, len(eng_sems[op.eng]))))
                    cnt[op.eng] = 0
                cnt[op.eng] += 1
                op.ticket = (eng_sems[op.eng][-1], cnt[op.eng])
        self.n_sems = nsem[0]
        with nc.Block() as block:
            for eng in self.ENGS:
                ops_e = [op for op in self.ops if op.eng == eng]

                def body(e, ops_e=ops_e, eng=eng):
                    wm = {}

                    def wait_for(dlist):
                        needw = {}
                        for d in dlist:
                            sem, val = d.ticket
                            k = id(sem)
                            if wm.get(k, 0) < val and needw.get(k, (None, 0))[1] < val:
                                needw[k] = (sem, val)
                        for k, (sem, val) in needw.items():
                            e.wait_ge(sem, val)
                            wm[k] = val

                    for op in ops_e:
                        wait_for(op.deps)
                        ins = op.fn(e)
                        if op.ticket is not None:
                            ins.then_inc(op.ticket[0], 16 if op.dma is not None else 1)
                    if eng == "sp":
                        wait_for(fin)

                getattr(block, self.BLK[eng])(body)


def pipeline(n, load, compute, depth=2):
    for s in range(min(depth, n)):
        load(s)
    for s in range(n):
        compute(s)
        if s + depth < n:
            load(s + depth)


def build(cfg, debug=False, stop_after=None, dry=False):
    c = cfg
    D, DC, TC, NT, E, FE, FC, NSLOT, NB, BLK, BS = c.D, c.DC, c.TC, c.NT, c.E, c.FE, c.FC, c.NSLOT, c.NB, c.BLK, c.BS
    RC, NBW = c.RC, c.NBW
    L = c.DEPTH
    LP, LG = (L + 1) // 2, L // 2
    nc = bass.Bass("TRN2", target_bir_lowering=False)
    stack = contextlib.ExitStack()

    def din(name, shape, dt=F32):
        return nc.dram_tensor(name, list(shape), dt, kind="ExternalInput").ap()

    def dscr(name, shape, dt):
        return nc.dram_tensor(name, list(shape), dt, kind="ExternalOutput" if debug else "Internal").ap()

    x_in = din("x", [TC, D])
    cT_in = din("cT", [128, DC])
    tokc_in = din("tokc", [5, TC])
    ident_in = din("ident", [128, 128])
    triu_in = din("triu", [128, 128])
    tril_in = din("tril", [128, 128])
    thr_in = din("thr", [1, NB])
    pcol_in = din("pcol", [128, 1])
    tokid_in = din("tokid", [128, NT * 2], I32)
    validT_in = din("validT", [128, NT])
    w_cond = din("w_cond", [D, c.RANK])
    b_condT = din("b_condT", [128, RC])
    w_mod = din("w_mod", [L, c.RANK, 6 * D])
    b_mod = din("b_mod", [L, 6 * D])
    g_mix = din("g_norm_mix", [L, D])
    g_ffn = din("g_norm_ffn", [L, D])
    p_win = din("pool_w_in", [LP, D, D])
    p_wg = din("pool_w_group", [LP, 4, c.GW, c.GW])
    p_scT = din("pool_scaleT", [LP, 128, DC])
    p_wout = din("pool_w_out", [LP, D, D])
    g_win = din("gmlp_w_in", [LG, D, 2 * D])
    g_binT = din("gmlp_b_inT", [LG, 128, DC])
    g_bin = din("gmlp_b_in", [LG, 2 * D])
    g_lngT = din("gmlp_ln_gT", [LG, 128, DC])
    g_lnbT = din("gmlp_ln_bT", [LG, 128, DC])
    g_ws = din("gmlp_w_s", [LG, c.HEADS, 128, 128])
    g_bs = din("gmlp_b_s", [LG, c.HEADS, 128])
    g_wout = din("gmlp_w_out", [LG, D, D])
    m_wr = din("moe_w_router", [L, D, E])
    m_br = din("moe_b_router", [L, E])
    m_wup = din("moe_w_upP", [L, E * 128, DC * 2 * FE])
    m_bupT = din("moe_b_upT", [L, E * 128, 2 * FC])
    m_wdn = din("moe_w_downP", [L, E * 128, FC * D])
    m_bdn = din("moe_b_down", [L, E, D])
    g_fin = din("g_final", [1, D])
    out = nc.dram_tensor("out", [c.SQ, D], F32, kind="ExternalOutput").ap()

    xres = dscr("xres", [TC, D], F32)
    hT = dscr("hT", [D, TC], BF16)
    uT = dscr("uT", [D, TC], BF16)
    mT = dscr("mT", [D, TC], BF16)
    vtok = dscr("vtok", [TC, D], BF16)
    hrows = dscr("hrows", [TC, D], BF16)
    ypairs = dscr("ypairs", [NSLOT + 128, D], BF16)
    slot_tok = dscr("slot_tok", [NSLOT, 2], I32)

    P = Prog(nc, stack, c.ARENA_KB, dry)
    ps = P.psum

    ident = P.persist([128, 128], BF16, "k_id")
    identf = P.persist([128, 128], F32, "k_idf")
    triu = P.persist([128, 128], F32, "k_tu")
    tril = P.persist([128, 128], F32, "k_tl")
    ones_f = P.persist([128, 128], F32)
    ones_b = P.persist([128, 128], BF16)
    THR = P.persist([128, NB], F32, "k_eo")
    pcol = P.persist([128, 1], F32, "k_pc")
    def _seg(n):
        sg = min(n, 2048)
        while n % sg:
            sg -= 1
        return sg

    UPR, DNR = DC * 2 * FE, FC * D
    su, sd, sb = _seg(UPR), _seg(DNR), _seg(D)
    nsu, nsd, nsb = UPR // su, DNR // sd, D // sb
    WIDX = P.persist([128, NB], I32)
    WIU = P.persist([128, NB, nsu], I32)
    WID = P.persist([128, NB, nsd], I32)
    BID = P.persist([128, NB, nsb], I32)
    tokid = P.persist([128, NT * 2], I32, "k_ti")
    validT = P.persist([128, NT], F32, "k_va")
    condT = P.persist([128, RC], F32)
    condB = P.persist([128, RC, 128], F32)
    IDX = P.persist([128, NT, 4], I32)
    GATE = P.persist([128, NT, 4], F32)
    S1 = P.persist([128, NT, D // NBW], F32)
    S2 = P.persist([128, NT, D // NBW], F32)
    zrow = P.persist([128, 512], BF16)

    def stage_const():
        P.stage()
        P.dma("pool", ident[:], ident_in, writes=[ident], key="k_id")
        P.dma("sp", identf[:], ident_in, writes=[identf], key="k_idf")
        P.dma("sp", triu[:], triu_in, writes=[triu], key="k_tu")
        P.dma("sp", tril[:], tril_in, writes=[tril], key="k_tl")
        P.dma("sp", THR[:], thr_in.partition_broadcast(128), writes=[THR], key="k_eo")
        P.dma("sp", pcol[:], pcol_in, writes=[pcol], key="k_pc")
        P.dma("sp", tokid[:], tokid_in, writes=[tokid], key="k_ti")
        P.dma("sp", validT[:], validT_in, writes=[validT], key="k_va")
        P.add("pool", lambda e: e.memset(ones_f[:], 1.0), writes=[ones_f])
        P.add("pool", lambda e: e.memset(ones_b[:], 1.0), writes=[ones_b])
        P.add("pool", lambda e: e.memset(zrow[:], 0.0), writes=[zrow])
        for q in range(D // 512):
            P.dma("sp", ypairs[NSLOT:NSLOT + 128, q * 512:(q + 1) * 512], zrow[:], reads=[zrow], key="k_z")
        wc = P.tile([128, DC, c.RANK], BF16, "w0")
        ct = P.tile([128, DC], BF16, "x0")
        bc = P.tile([128, RC], F32, "x1")
        P.dma("pool", wc[:], w_cond.rearrange("(kc p) r -> p kc r", p=128), writes=[wc], key="w0")
        P.dma("pool", ct[:], cT_in, writes=[ct], key="x0")
        P.dma("sp", bc[:], b_condT, writes=[bc], key="x1")
        for rc in range(RC):
            for kc in range(DC):
                P.add("pe", lambda e, rc=rc, kc=kc: e.matmul(ps[6][:, rc:rc + 1], lhsT=wc[:, kc, rc * 128:(rc + 1) * 128],
                                                              rhs=ct[:, kc:kc + 1], start=(kc == 0), stop=(kc == DC - 1)),
                      reads=[wc, ct], writes=[ps[6]])
        for rc in range(RC):
            P.add("act", lambda e, rc=rc: e.activation(out=condT[:, rc:rc + 1], in_=ps[6][:, rc:rc + 1], func=AF.Silu,
                                                       bias=bc[:, rc:rc + 1], scale=1.0),
                  reads=[ps[6], bc], writes=[condT])
        for rc in range(RC):
            P.add("dve", lambda e, rc=rc: e.tensor_scalar(out=condB[:, rc, :], in0=ones_f[:], scalar1=condT[:, rc:rc + 1],
                                                          scalar2=None, op0=ALU.mult),
                  reads=[ones_f, condT], writes=[condB])

    def emit_mod(l, which, want):
        gsrc = (g_mix if which == 0 else g_ffn)
        gb = None
        if any(v == 1 for v, _ in want):
            gb = P.tile([128, D], F32, "m_g")
            P.dma("sp", gb[:], gsrc[l:l + 1, :].partition_broadcast(128), writes=[gb], key="m_g")
        wms = [P.tile([128, RC, 512], F32, "m_w%d" % i) for i in range(2)]
        bbs = [P.tile([128, 512], F32, "m_b%d" % i) for i in range(2)]
        tmp = P.tile([128, 512], F32)
        items = [(v, dst, nb) for v, dst in want for nb in range(D // NBW)]

        def load(s):
            v, dst, nb = items[s]
            col = (which * 3 + v) * D + nb * NBW
            wm, bb = wms[s % 2], bbs[s % 2]
            P.dma("sp", wm[:, :, :NBW], w_mod[l, :, col:col + NBW].rearrange("(rc p) n -> p rc n", p=128),
                  writes=[wm], key=wm.dkey)
            P.dma("sp", bb[:, :NBW], b_mod[l:l + 1, col:col + NBW].partition_broadcast(128), writes=[bb], key=bb.dkey)

        def compute(s):
            v, dst, nb = items[s]
            wm, bb = wms[s % 2], bbs[s % 2]
            pt = ps[s % 2]
            cs = slice(nb * NBW, (nb + 1) * NBW)
            for rc in range(RC):
                P.add("pe", lambda e, rc=rc: e.matmul(pt[:, :NBW], lhsT=condB[:, rc, :], rhs=wm[:, rc, :NBW],
                                                      start=(rc == 0), stop=(rc == RC - 1)),
                      reads=[condB, wm], writes=[pt])
            if v == 1:
                P.add("dve", lambda e: e.tensor_tensor(out=tmp[:, :NBW], in0=pt[:, :NBW], in1=bb[:, :NBW], op=ALU.add),
                      reads=[pt, bb], writes=[tmp])
                P.add("dve", lambda e: e.scalar_tensor_tensor(out=dst[:, cs], in0=tmp[:, :NBW], scalar=1.0, in1=gb[:, cs],
                                                               op0=ALU.add, op1=ALU.mult),
                      reads=[tmp, gb], writes=[dst])
            else:
                P.add("dve", lambda e: e.tensor_tensor(out=dst[:, cs], in0=pt[:, :NBW], in1=bb[:, :NBW], op=ALU.add),
                      reads=[pt, bb], writes=[dst])

        pipeline(len(items), load, compute)

    def stage_norm(l, which, src, moe):
        P.stage()
        MA = P.tile([128, D], F32)
        MB = P.tile([128, D], F32)
        mark = P.sp_off
        emit_mod(l, which, [(1, MA), (0, MB)])
        P.stage(keep=mark)
        xts = [P.tile([128, D], F32, "x%d" % i) for i in range(2)]
        t1 = P.tile([128, D], F32)
        hbs = [P.tile([128, D], BF16, "h%d" % i) for i in range(2)]
        htss = [P.tile([128, DC, 256], BF16, "o%d" % i) for i in range(2)]
        sss = [P.tile([128, 4], F32) for i in range(2)]
        if moe:
            wr = P.tile([128, DC, E], BF16, "w0")
            brr = P.tile([1, E], BF16, "w1")
            P.dma("pool", wr[:], m_wr[l].rearrange("(kc p) e -> p kc e", p=128), writes=[wr], key="w0")
            P.dma("pool", brr[:], m_br[l:l + 1, :], writes=[brr], key="w1")
            bases = [P.tile([1, E], F32) for i in range(2)]
            P.add("pool", lambda e: e.memset(bases[0][:], 0.0), writes=[bases[0]])
            P.add("pool", lambda e: e.memset(GATE[:], 0.0), writes=[GATE])
            sm = {n: P.tile([128, E], F32) for n in ("lg", "mk", "ex", "pm", "gts", "vl", "k1", "key", "oh", "gv", "jk")}
            t8 = P.tile([128, 8], F32)
            k8 = P.tile([128, 8], F32)
            RK = P.tile([128, NT, E], F32)
            MK = P.tile([128, NT, E], F32)
            GV = P.tile([128, NT, E], F32)
            s4 = {n: P.tile([128, 4], F32) for n in ("nm", "z4", "f4")}

        def load(i):
            P.dma("sp", xts[i % 2][:], src[i * 128:(i + 1) * 128, :], writes=[xts[i % 2]], key=xts[i % 2].dkey)

        def compute(i):
            xt, hb, ss = xts[i % 2], hbs[i % 2], sss[i % 2]
            g4, sub = i // 2, i % 2
            hts = htss[g4 % 2]
            P.add("act", lambda e: e.activation(out=hb[:], in_=xt[:], func=AF.Square, accum_out=ss[:, 0:1]),
                  reads=[xt], writes=[hb, ss])
            P.add("act", lambda e: e.activation(out=ss[:, 1:2], in_=ss[:, 0:1], func=AF.Sqrt, bias=1e-5, scale=1.0 / D),
                  reads=[ss], writes=[ss])
            P.add("dve", lambda e: e.reciprocal(out=ss[:, 2:3], in_=ss[:, 1:2]), reads=[ss], writes=[ss])
            P.add("dve", lambda e: e.scalar_tensor_tensor(out=t1[:], in0=xt[:], scalar=ss[:, 2:3], in1=MA[:],
                                                           op0=ALU.mult, op1=ALU.mult),
                  reads=[xt, ss, MA], writes=[t1])
            P.add("pool", lambda e: e.tensor_tensor(out=hb[:], in0=t1[:], in1=MB[:], op=ALU.add),
                  reads=[t1, MB], writes=[hb])
            if moe:
                P.dma("sp", hrows[i * 128:(i + 1) * 128, :], hb[:], reads=[hb], key=hb.dkey)
            for c8 in range(0, DC, 8):
                pt = ps[4 + (c8 // 8) % 2]
                ptb = pt[:].bitcast(BF16)
                for cc in range(8):
                    ch = c8 + cc
                    P.add("pe", lambda e, ch=ch, cc=cc, ptb=ptb: e.transpose(out=ptb[:, cc * 128:(cc + 1) * 128],
                                                                              in_=hb[:, ch * 128:(ch + 1) * 128],
                                                                              identity=ident[:]),
                          reads=[hb, ident], writes=[pt])
                eng = "act" if (c8 // 8) % 2 == 0 else "dve"
                src_v = ptb.rearrange("p (a b) -> p a b", a=8)
                dst_v = hts[:, c8:c8 + 8, sub * 128:(sub + 1) * 128]
                if eng == "act":
                    P.add("act", lambda e, src_v=src_v, dst_v=dst_v: e.copy(out=dst_v, in_=src_v), reads=[pt], writes=[hts])
                else:
                    P.add("dve", lambda e, src_v=src_v, dst_v=dst_v: e.tensor_copy(out=dst_v, in_=src_v), reads=[pt], writes=[hts])
            if moe:
                emit_router(i, hts, sub)
            if sub == 1 or i == NT - 1:
                t0 = g4 * 256
                tw = (sub + 1) * 128
                if not moe:
                    P.dma("sp", hT[:, t0:t0 + tw].rearrange("(c p) t -> p c t", p=128), hts[:, :, :tw], reads=[hts], key=hts.dkey)

        def emit_router(i, hts, sub):
            pr, pk, pb = ps[6], ps[7], ps[6]
            lg, mk, ex, pm, gts, vl, k1, key, oh, gv, jk = (sm[n] for n in ("lg", "mk", "ex", "pm", "gts", "vl", "k1", "key", "oh", "gv", "jk"))
            nm, z4, f4 = s4["nm"], s4["z4"], s4["f4"]
            b_old, b_new = bases[i % 2], bases[(i + 1) % 2]
            for kc in range(DC):
                P.add("pe", lambda e, kc=kc: e.matmul(pr[:, 0:E], lhsT=hts[:, kc, sub * 128:(sub + 1) * 128], rhs=wr[:, kc, :],
                                                      start=(kc == 0), stop=False),
                      reads=[hts, wr], writes=[pr])
            P.add("pe", lambda e: e.matmul(pr[:, 0:E], lhsT=ones_b[0:1, :], rhs=brr[0:1, :], start=False, stop=True),
                  reads=[ones_b, brr], writes=[pr])
            P.add("dve", lambda e: e.tensor_copy(out=lg[:], in_=pr[:, 0:E]), reads=[pr], writes=[lg])
            P.add("dve", lambda e: e.max(out=t8[:], in_=lg[:]), reads=[lg], writes=[t8])
            P.add("dve", lambda e: e.tensor_scalar(out=mk[:], in0=lg[:], scalar1=t8[:, 3:4], scalar2=validT[:, i:i + 1], op0=ALU.is_ge, op1=ALU.mult),
                  reads=[lg, t8, validT], writes=[mk])
            P.add("dve", lambda e: e.tensor_scalar(out=nm[:, 0:1], in0=t8[:, 0:1], scalar1=-1.0, scalar2=None, op0=ALU.mult),
                  reads=[t8], writes=[nm])
            P.add("act", lambda e: e.activation(out=ex[:], in_=lg[:], func=AF.Exp, bias=nm[:, 0:1], scale=1.0),
                  reads=[lg, nm], writes=[ex])
            P.add("dve", lambda e: e.scalar_tensor_tensor(out=pm[:], in0=ex[:], scalar=1.0, in1=mk[:], op0=ALU.mult, op1=ALU.mult,
                                                           accum_out=nm[:, 1:2]),
                  reads=[ex, mk], writes=[pm, nm])
            P.add("dve", lambda e: e.tensor_scalar(out=nm[:, 3:4], in0=nm[:, 1:2], scalar1=1e-30, scalar2=None, op0=ALU.add), reads=[nm], writes=[nm])
            P.add("dve", lambda e: e.reciprocal(out=nm[:, 2:3], in_=nm[:, 3:4]), reads=[nm], writes=[nm])
            P.add("dve", lambda e: e.tensor_scalar(out=gts[:], in0=pm[:], scalar1=nm[:, 2:3], scalar2=None, op0=ALU.mult),
                  reads=[pm, nm], writes=[gts])
            P.add("pe", lambda e: e.matmul(pk[:, 0:E], lhsT=triu[:], rhs=mk[:], start=True, stop=False), reads=[triu, mk], writes=[pk])
            P.add("pe", lambda e: e.matmul(pk[:, 0:E], lhsT=ones_f[0:1, :], rhs=b_old[0:1, :], start=False, stop=True),
                  reads=[ones_f, b_old], writes=[pk])
            P.add("pe", lambda e: e.matmul(pb[0:1, 64:64 + E], lhsT=ones_f[:, 0:1], rhs=mk[:], start=True, stop=False),
                  reads=[ones_f, mk, lg], writes=[pb])
            P.add("pe", lambda e: e.matmul(pb[0:1, 64:64 + E], lhsT=ones_f[0:1, 0:1], rhs=b_old[0:1, :], start=False, stop=True),
                  reads=[ones_f, b_old], writes=[pb])
            P.add("dve", lambda e: e.tensor_copy(out=b_new[:], in_=pb[0:1, 64:64 + E]), reads=[pb], writes=[b_new])
            P.add("dve", lambda e: e.tensor_copy(out=RK[:, i, :], in_=pk[:, 0:E]), reads=[pk], writes=[RK])
            P.add("act", lambda e: e.copy(out=MK[:, i, :], in_=mk[:]), reads=[mk], writes=[MK])
            P.add("act", lambda e: e.copy(out=GV[:, i, :], in_=gts[:]), reads=[gts], writes=[GV])

        def emit_dispatch():
            cnt = bases[NT % 2]
            r1 = P.tile([1, 2 * E], F32)
            r1i = P.tile([1, 2 * E], I32)
            sc = [P.tile([1, 2 * E], F32) for _ in range(2)]
            PB = P.tile([128, 2 * E], F32)
            EB = P.tile([128, NB], F32)
            EB2 = P.tile([128, NB], F32)
            pq = ps[7]
            P.add("dve", lambda e: e.tensor_scalar(out=r1[:, 0:E], in0=cnt[:], scalar1=BLK / 2 - 0.5, scalar2=1.0 / BLK, op0=ALU.add, op1=ALU.mult),
                  reads=[cnt], writes=[r1])
            P.add("dve", lambda e: e.tensor_copy(out=r1i[:, 0:E], in_=r1[:, 0:E]), reads=[r1], writes=[r1i])
            P.add("dve", lambda e: e.tensor_copy(out=r1[:, 0:E], in_=r1i[:, 0:E]), reads=[r1i], writes=[r1])
            P.add("dve", lambda e: e.memset(sc[0][:], 0.0), writes=[sc[0]])
            P.add("dve", lambda e: e.memset(sc[1][:], 0.0), writes=[sc[1]])
            P.add("dve", lambda e: e.tensor_scalar(out=sc[0][:, E:2 * E], in0=r1[:, 0:E], scalar1=float(BLK), scalar2=None, op0=ALU.mult),
                  reads=[r1], writes=[sc[0]])
            P.add("dve", lambda e: e.tensor_copy(out=r1[:, E:2 * E], in_=sc[0][:, E:2 * E]), reads=[sc[0]], writes=[r1])
            cur = 0
            sh = 1
            while sh < E:
                a, b = sc[cur], sc[1 - cur]
                P.add("dve", lambda e, a=a, b=b, sh=sh: e.tensor_tensor(out=b[:, E:2 * E], in0=a[:, E:2 * E], in1=a[:, E - sh:2 * E - sh], op=ALU.add),
                      reads=[a], writes=[b])
                cur = 1 - cur
                sh *= 2
            pend = sc[cur]
            P.add("dve", lambda e: e.tensor_tensor(out=r1[:, 0:E], in0=pend[:, E:2 * E], in1=r1[:, E:2 * E], op=ALU.subtract), reads=[pend, r1], writes=[r1])
            P.add("dve", lambda e: e.tensor_copy(out=r1[:, E:2 * E], in_=pend[:, E:2 * E]), reads=[pend], writes=[r1])
            P.add("pe", lambda e: e.matmul(pq[:, 0:2 * E], lhsT=ones_f[0:1, :], rhs=r1[0:1, :], start=True, stop=True), reads=[ones_f, r1], writes=[pq])
            P.add("dve", lambda e: e.tensor_copy(out=PB[:], in_=pq[:, 0:2 * E]), reads=[pq], writes=[PB])
            P.add("dve", lambda e: e.memset(EB[:], 0.0), writes=[EB])
            cur_e, oth_e = EB, EB2
            for ex_ in range(E):
                P.add("dve", lambda e, ex_=ex_, cur_e=cur_e, oth_e=oth_e: e.scalar_tensor_tensor(
                    out=oth_e[:], in0=THR[:], scalar=PB[:, E + ex_:E + ex_ + 1], in1=cur_e[:], op0=ALU.is_ge, op1=ALU.add),
                    reads=[THR, PB, cur_e], writes=[oth_e])
                cur_e, oth_e = oth_e, cur_e
            P.add("dve", lambda e, cur_e=cur_e, oth_e=oth_e: e.tensor_scalar(out=oth_e[:], in0=cur_e[:], scalar1=float(E - 1), scalar2=None, op0=ALU.min),
                  reads=[cur_e], writes=[oth_e])
            ebf = oth_e
            P.add("dve", lambda e, cur_e=cur_e: e.tensor_scalar(out=cur_e[:], in0=ebf[:], scalar1=float(l * E), scalar2=None, op0=ALU.add),
                  reads=[ebf], writes=[cur_e])
            tmpf = P.tile([128, NB], F32)
            for a in range(nsb):
                P.add("dve", lambda e, a=a, cur_e=cur_e: e.tensor_scalar(out=tmpf[:], in0=cur_e[:], scalar1=float(nsb), scalar2=float(a), op0=ALU.mult, op1=ALU.add),
                      reads=[cur_e], writes=[tmpf])
                P.add("dve", lambda e, a=a: e.tensor_copy(out=BID[:, :, a], in_=tmpf[:]), reads=[tmpf], writes=[BID])
            P.add("dve", lambda e, cur_e=cur_e: e.tensor_scalar(out=ebf[:], in0=cur_e[:], scalar1=128.0, scalar2=pcol[:, 0:1], op0=ALU.mult, op1=ALU.add),
                  reads=[cur_e, pcol], writes=[ebf])
            P.add("dve", lambda e: e.tensor_copy(out=WIDX[:], in_=ebf[:]), reads=[ebf], writes=[WIDX])
            for (ns_, dst_) in ((nsu, WIU), (nsd, WID)):
                for a in range(ns_):
                    P.add("dve", lambda e, a=a, ns_=ns_: e.tensor_scalar(out=tmpf[:], in0=ebf[:], scalar1=float(ns_), scalar2=float(a), op0=ALU.mult, op1=ALU.add),
                          reads=[ebf], writes=[tmpf])
                    P.add("dve", lambda e, a=a, dst_=dst_: e.tensor_copy(out=dst_[:, :, a], in_=tmpf[:]), reads=[tmpf], writes=[dst_])
            key, oh, jk, k1 = sm["key"], sm["oh"], sm["jk"], sm["k1"]
            z4, f4 = s4["z4"], s4["f4"]
            for i in range(NT):
                P.add("dve", lambda e, i=i: e.scalar_tensor_tensor(out=k1[:], in0=RK[:, i, :], scalar=1.0, in1=PB[:, 0:E], op0=ALU.add, op1=ALU.add),
                      reads=[RK, PB], writes=[k1])
                P.add("dve", lambda e, i=i: e.tensor_tensor(out=key[:], in0=k1[:], in1=MK[:, i, :], op=ALU.mult), reads=[k1, MK], writes=[key])
                P.add("dve", lambda e: e.max(out=k8[:], in_=key[:]), reads=[key], writes=[k8])
                P.add("dve", lambda e: e.tensor_scalar(out=z4[:], in0=k8[:, 0:4], scalar1=0.0, scalar2=float(NSLOT + 1),
                                                       op0=ALU.is_equal, op1=ALU.mult), reads=[k8], writes=[z4])
                P.add("dve", lambda e: e.scalar_tensor_tensor(out=f4[:], in0=k8[:, 0:4], scalar=-1.0, in1=z4[:], op0=ALU.add, op1=ALU.add),
                      reads=[k8, z4], writes=[f4])
                P.add("dve", lambda e, i=i: e.tensor_copy(out=IDX[:, i, :], in_=f4[:]), reads=[f4], writes=[IDX])
                for k in range(4):
                    P.add("dve", lambda e, k=k: e.tensor_scalar(out=oh[:], in0=key[:], scalar1=k8[:, k:k + 1], scalar2=None, op0=ALU.is_equal),
                          reads=[key, k8], writes=[oh])
                    P.add("dve", lambda e, k=k, i=i: e.scalar_tensor_tensor(out=jk[:], in0=oh[:], scalar=1.0, in1=GV[:, i, :], op0=ALU.mult, op1=ALU.mult,
                                                                            accum_out=GATE[:, i, k:k + 1]),
                          reads=[oh, GV], writes=[jk, GATE])
                for k in range(4):
                    P.add("pool", lambda e, k=k, i=i: e.indirect_dma_start(
                        out=slot_tok[:, :], out_offset=bass.IndirectOffsetOnAxis(ap=IDX[:, i, k:k + 1], axis=0),
                        in_=tokid[:, 2 * i:2 * i + 2], in_offset=None, bounds_check=P.reg(e, NSLOT - 1), oob_is_err=False),
                        reads=[IDX, tokid], dma="k_sc%d" % k)

        pipeline(NT, load, compute)
        if moe:
            emit_dispatch()

    def gemm_fm(W, X, KC, N, epi, post, extra_load=None):
        nbw = min(512, N)
        mch = nbw // 128
        nnb = N // nbw
        wbs = [P.tile([128, KC, nbw], BF16, "w%d" % i) for i in range(2)]
        xbs = [P.tile([128, KC, 512], BF16, "x%d" % i) for i in range(2)]
        items = [(nbi, ti) for nbi in range(nnb) for ti in range(len(c.ttiles))]

        def loadw(nbi):
            wb = wbs[nbi % 2]
            P.dma("pool", wb[:], W[:, nbi * nbw:(nbi + 1) * nbw].rearrange("(kc p) n -> p kc n", p=128), writes=[wb], key=wb.dkey)

        def load(s):
            nbi, ti = items[s]
            t0, tw = c.ttiles[ti]
            xb = xbs[s % 2]
            P.dma("sp", xb[:, :, :tw], X[:, t0:t0 + tw].rearrange("(kc p) t -> p kc t", p=128), writes=[xb], key=xb.dkey)
            if extra_load is not None:
                extra_load(s, nbi, ti, t0, tw)

        def compute(s):
            nbi, ti = items[s]
            t0, tw = c.ttiles[ti]
            wb, xb = wbs[nbi % 2], xbs[s % 2]
            for m in range(mch):
                pt = ps[(s * mch + m) % 4]
                for kc in range(KC):
                    P.add("pe", lambda e, kc=kc, m=m, pt=pt: e.matmul(pt[:, :tw], lhsT=wb[:, kc, m * 128:(m + 1) * 128], rhs=xb[:, kc, :tw],
                                                                   start=(kc == 0), stop=(kc == KC - 1)),
                          reads=[wb, xb], writes=[pt])
                epi(s, nbi, m, ti, t0, tw, pt)
            post(s, nbi, ti, t0, tw)
            if ti == len(c.ttiles) - 1 and nbi + 2 < nnb:
                loadw(nbi + 2)

        for nbi in range(min(2, nnb)):
            loadw(nbi)
        pipeline(len(items), load, compute)

    def gemm_tm(W, X, KC, N, bias, epi, post, extra_load=None):
        nbw = min(512, N)
        nnb = N // nbw
        wbs = [P.tile([128, KC, nbw], BF16, "w%d" % i) for i in range(2)]
        xbs = [P.tile([128, KC, 512], BF16, "x%d" % i) for i in range(2)]
        brs = [P.tile([1, nbw], BF16, "b%d" % i) for i in range(2)] if bias is not None else None
        items = [(nbi, ti) for nbi in range(nnb) for ti in range(len(c.ttiles))]

        def loadw(nbi):
            wb = wbs[nbi % 2]
            P.dma("pool", wb[:], W[:, nbi * nbw:(nbi + 1) * nbw].rearrange("(kc p) n -> p kc n", p=128), writes=[wb], key=wb.dkey)
            if bias is not None:
                br = brs[nbi % 2]
                P.dma("pool", br[:], bias[:, nbi * nbw:(nbi + 1) * nbw], writes=[br], key=br.dkey)

        def load(s):
            nbi, ti = items[s]
            t0, tw = c.ttiles[ti]
            xb = xbs[s % 2]
            P.dma("sp", xb[:, :, :tw], X[:, t0:t0 + tw].rearrange("(kc p) t -> p kc t", p=128), writes=[xb], key=xb.dkey)
            if extra_load is not None:
                extra_load(s, nbi, ti, t0, tw)

        def compute(s):
            nbi, ti = items[s]
            t0, tw = c.ttiles[ti]
            wb, xb = wbs[nbi % 2], xbs[s % 2]
            for sub in range(tw // 128):
                pt = ps[(s * 4 + sub) % 4]
                for kc in range(KC):
                    P.add("pe", lambda e, kc=kc, sub=sub, pt=pt: e.matmul(pt[:, :nbw], lhsT=xb[:, kc, sub * 128:(sub + 1) * 128], rhs=wb[:, kc, :],
                                                                       start=(kc == 0), stop=(kc == KC - 1 and bias is None)),
                          reads=[wb, xb], writes=[pt])
                if bias is not None:
                    br = brs[nbi % 2]
                    P.add("pe", lambda e, pt=pt, br=br: e.matmul(pt[:, :nbw], lhsT=ones_b[0:1, :], rhs=br[0:1, :], start=False, stop=True),
                          reads=[ones_b, br], writes=[pt])
                epi(s, nbi, ti, sub, t0 // 128 + sub, pt, nbw)
            post(s, nbi, ti, t0, tw, nbw)
            if ti == len(c.ttiles) - 1 and nbi + 2 < nnb:
                loadw(nbi + 2)

        for nbi in range(min(2, nnb)):
            loadw(nbi)
        pipeline(len(items), load, compute)

    def stage_out_gemm(l, W, xsrc):
        P.stage()
        MG = P.tile([128, D], F32)
        mark = P.sp_off
        emit_mod(l, 0, [(2, MG)])
        P.stage(keep=mark)
        xfs = [P.tile([128, 4, 512], F32, "r%d" % i) for i in range(2)]
        xos = xfs
        tmp = [P.tile([128, 512], F32) for i in range(2)]

        def extra_load(s, nbi, ti, t0, tw):
            xf = xfs[s % 2]
            P.dma("sp", xf[:, :tw // 128, :NBW], xsrc[t0:t0 + tw, nbi * NBW:(nbi + 1) * NBW].rearrange("(a p) n -> p a n", p=128),
                  writes=[xf], key=xf.dkey)

        def epi(s, nbi, ti, sub, itile, pt, nbw):
            xf, xo, tm = xfs[s % 2], xos[s % 2], tmp[sub % 2]
            P.add("dve", lambda e: e.tensor_tensor(out=tm[:, :nbw], in0=pt[:, :nbw], in1=MG[:, nbi * nbw:(nbi + 1) * nbw], op=ALU.mult),
                  reads=[pt, MG], writes=[tm])
            P.add("pool", lambda e: e.tensor_tensor(out=xo[:, sub, :nbw], in0=tm[:, :nbw], in1=xf[:, sub, :nbw], op=ALU.add),
                  reads=[tm, xf], writes=[xo])

        def post(s, nbi, ti, t0, tw, nbw):
            xo = xos[s % 2]
            P.dma("sp", xres[t0:t0 + tw, nbi * nbw:(nbi + 1) * nbw].rearrange("(a p) n -> p a n", p=128), xo[:, :tw // 128, :nbw],
                  reads=[xo], key=xo.dkey)

        gemm_tm(W, mT, DC, D, None, epi, post, extra_load)

    def stage_pool_in(j):
        P.stage()
        GC = DC // 4
        tcs = [P.tile([128, 5, 512], F32, "c%d" % i) for i in range(2)]
        obs = [P.tile([128, 4, 512], BF16, "o%d" % i) for i in range(2)]
        ues = [[P.tile([128, 16 + 512], F32) for par in range(2)] for m in range(4)]
        sA = [P.tile([128, 16 + 512], F32) for i in range(2)]
        sB = [P.tile([128, 16 + 512], F32) for i in range(2)]
        for m in range(4):
            P.add("pool", lambda e, m=m: e.memset(ues[m][0][:, 0:16], 0.0), writes=[ues[m][0]])

        def extra_load(s, nbi, ti, t0, tw):
            tcn = tcs[s % 2]
            P.dma("sp", tcn[:, :, :tw], tokc_in[:, t0:t0 + tw].partition_broadcast(128), writes=[tcn], key=tcn.dkey)

        def epi(s, nbi, m, ti, t0, tw, pt):
            fc = nbi * (NBW // 128) + m
            g = fc // GC
            tcn, ob = tcs[s % 2], obs[s % 2]
            ue, uen = ues[m][ti % 2], ues[m][(ti + 1) % 2]
            a, b = sA[m % 2], sB[m % 2]
            Wd = 16 + tw
            if ti == 0 and nbi > 0:
                P.add("pool", lambda e: e.memset(ue[:, 0:16], 0.0), writes=[ue])
            P.add("dve", lambda e: e.tensor_tensor(out=ue[:, 16:Wd], in0=pt[:, :tw], in1=tcn[:, 0, :tw], op=ALU.mult),
                  reads=[pt, tcn], writes=[ue])
            if ti + 1 < len(c.ttiles):
                P.add("act", lambda e: e.copy(out=uen[:, 0:16], in_=ue[:, tw:tw + 16]), reads=[ue], writes=[uen])
            bufs = [ue, a, b, a, b]
            for st in range(g + 1):
                sh = 1 << st
                lo = 2 * sh - 1
                src_t, dst_t = bufs[st], bufs[st + 1]
                P.add("pool", lambda e, src_t=src_t, dst_t=dst_t, sh=sh, lo=lo: e.tensor_tensor(
                    out=dst_t[:, lo:Wd], in0=src_t[:, lo:Wd], in1=src_t[:, lo - sh:Wd - sh], op=ALU.add),
                    reads=[src_t], writes=[dst_t])
            res = bufs[g + 1]
            oth = b if res is a else a
            P.add("dve", lambda e: e.tensor_tensor(out=oth[:, 16:Wd], in0=res[:, 16:Wd], in1=tcn[:, 1 + g, :tw], op=ALU.mult),
                  reads=[res, tcn], writes=[oth])
            P.add("dve", lambda e: e.tensor_tensor(out=ob[:, m, :tw], in0=oth[:, 16:Wd], in1=ue[:, 16:Wd], op=ALU.subtract),
                  reads=[oth, ue], writes=[ob])

        def post(s, nbi, ti, t0, tw):
            ob = obs[s % 2]
            mch = NBW // 128
            P.dma("sp", uT[nbi * NBW:(nbi + 1) * NBW, t0:t0 + tw].rearrange("(m p) t -> p m t", p=128), ob[:, :mch, :tw],
                  reads=[ob], key=ob.dkey)

        gemm_fm(p_win[j], hT, DC, D, epi, post, extra_load)

    def stage_pool_group(j):
        P.stage()
        GW = c.GW
        KCg = GW // 128
        psc = P.tile([128, DC], F32, "c0")
        P.dma("sp", psc[:], p_scT[j], writes=[psc], key="c0")
        obs = [P.tile([128, 4, 512], BF16, "o%d" % i) for i in range(2)]
        for g in range(4):
            nbw = min(512, GW)

            def epi(s, nbi, m, ti, t0, tw, pt, g=g, nbw=nbw):
                fc = (g * GW + nbi * nbw) // 128 + m
                ob = obs[s % 2]
                if m % 2 == 0:
                    P.add("dve", lambda e: e.tensor_scalar(out=ob[:, m, :tw], in0=pt[:, :tw], scalar1=psc[:, fc:fc + 1], scalar2=None, op0=ALU.mult),
                          reads=[pt, psc], writes=[ob])
                else:
                    P.add("act", lambda e: e.activation(out=ob[:, m, :tw], in_=pt[:, :tw], func=AF.Copy, scale=psc[:, fc:fc + 1]),
                          reads=[pt, psc], writes=[ob])

            def post(s, nbi, ti, t0, tw, g=g, nbw=nbw):
                ob = obs[s % 2]
                r0 = g * GW + nbi * nbw
                P.dma("sp", mT[r0:r0 + nbw, t0:t0 + tw].rearrange("(m p) t -> p m t", p=128), ob[:, :nbw // 128, :tw],
                      reads=[ob], key=ob.dkey)

            gemm_fm(p_wg[j, g], uT[g * GW:(g + 1) * GW, :], KCg, GW, epi, post)

    def stage_gmlp_u(j):
        P.stage()
        bT = P.tile([128, DC], F32, "c0")
        P.dma("sp", bT[:], g_binT[j], writes=[bT], key="c0")
        obs = [P.tile([128, 4, 512], BF16, "o%d" % i) for i in range(2)]

        def epi(s, nbi, m, ti, t0, tw, pt):
            fc = nbi * (NBW // 128) + m
            ob = obs[s % 2]
            P.add("act", lambda e: e.activation(out=ob[:, m, :tw], in_=pt[:, :tw], func=AF.Gelu, bias=bT[:, fc:fc + 1], scale=1.0),
                  reads=[pt, bT], writes=[ob])

        def post(s, nbi, ti, t0, tw):
            ob = obs[s % 2]
            P.dma("sp", uT[nbi * NBW:(nbi + 1) * NBW, t0:t0 + tw].rearrange("(m p) t -> p m t", p=128), ob[:, :NBW // 128, :tw],
                  reads=[ob], key=ob.dkey)

        gemm_fm(g_win[j, :, 0:D], hT, DC, D, epi, post)

    def stage_gmlp_v(j):
        P.stage()
        P.add("pool", lambda e: e.memset(S1[:], 0.0), writes=[S1])
        P.add("pool", lambda e: e.memset(S2[:], 0.0), writes=[S2])
        vfs = [P.tile([128, 512], F32) for i in range(2)]
        junk = P.tile([128, 512], BF16)
        vbs = [P.tile([128, 4, 512], BF16, "o%d" % i) for i in range(2)]

        def epi(s, nbi, ti, sub, itile, pt, nbw):
            vf, vb = vfs[sub % 2], vbs[s % 2]
            P.add("act", lambda e: e.activation(out=vf[:, :nbw], in_=pt[:, :nbw], func=AF.Gelu, accum_out=S1[:, itile, nbi:nbi + 1]),
                  reads=[pt], writes=[vf, S1])
            P.add("act", lambda e: e.activation(out=junk[:, :nbw], in_=vf[:, :nbw], func=AF.Square, accum_out=S2[:, itile, nbi:nbi + 1]),
                  reads=[vf], writes=[junk, S2])
            P.add("pool", lambda e: e.tensor_copy(out=vb[:, sub, :nbw], in_=vf[:, :nbw]), reads=[vf], writes=[vb])

        def post(s, nbi, ti, t0, tw, nbw):
            vb = vbs[s % 2]
            P.dma("sp", vtok[t0:t0 + tw, nbi * nbw:(nbi + 1) * nbw].rearrange("(a p) n -> p a n", p=128), vb[:, :tw // 128, :nbw],
                  reads=[vb], key=vb.dkey)

        gemm_tm(g_win[j, :, D:2 * D], hT, DC, D, g_bin[j:j + 1, D:2 * D], epi, post)

    def stage_gmlp_spatial(j):
        P.stage()
        H = c.HEADS
        CPH = DC // H
        lng = P.tile([128, DC], F32, "c0")
        lnb = P.tile([128, DC], F32, "c1")
        P.dma("sp", lng[:], g_lngT[j], writes=[lng], key="c0")
        P.dma("sp", lnb[:], g_lnbT[j], writes=[lnb], key="c1")
        bsb = P.tile([128, H, 128], F32, "b0")
        P.dma("sp", bsb[:], g_bs[j].partition_broadcast(128), writes=[bsb], key="b0")
        WcT = P.tile([128, H, 128], BF16)
        Et = P.tile([128, DC, 128], F32)
        wls = [P.tile([128, 128], F32, "r%d" % i) for i in range(2)]
        wms = [P.tile([128, 128], BF16) for i in range(2)]
        for h in range(H):
            wl, wm = wls[h % 2], wms[h % 2]
            pt = ps[4 + h % 2]
            ptb = pt[:].bitcast(BF16)
            P.dma("sp", wl[:], g_ws[j, h], writes=[wl], key=wl.dkey)
            P.add("dve", lambda e, wl=wl, wm=wm: e.tensor_tensor(out=wm[:], in0=wl[:], in1=tril[:], op=ALU.mult), reads=[wl, tril], writes=[wm])
            P.add("pe", lambda e, wm=wm, ptb=ptb: e.transpose(out=ptb[:, 0:128], in_=wm[:], identity=ident[:]), reads=[wm, ident], writes=[pt])
            P.add("act", lambda e, h=h, ptb=ptb: e.copy(out=WcT[:, h, :], in_=ptb[:, 0:128]), reads=[pt], writes=[WcT])
        for h in range(H):
            pt = ps[6 + h % 2]
            P.add("pe", lambda e, h=h, pt=pt: e.matmul(pt[:, 0:128], lhsT=ones_b[:], rhs=WcT[:, h, :], start=True, stop=True),
                  reads=[ones_b, WcT], writes=[pt])
            for cc in range(CPH):
                fc = h * CPH + cc
                P.add("dve", lambda e, h=h, fc=fc, pt=pt: e.scalar_tensor_tensor(out=Et[:, fc, :], in0=pt[:, 0:128], scalar=lnb[:, fc:fc + 1],
                                                                              in1=bsb[:, h, :], op0=ALU.mult, op1=ALU.add),
                      reads=[pt, lnb, bsb], writes=[Et])
        ubs = [P.tile([128, DC, 512], BF16, "x%d" % i) for i in range(2)]
        gos = [P.tile([128, DC, 512], BF16, "o%d" % i) for i in range(2)]
        vts = [P.tile([128, D], BF16, "h%d" % i) for i in range(2)]
        vns = [P.tile([128, D], BF16) for i in range(2)]
        sts = [P.tile([128, 8], F32) for i in range(2)]
        tmps = [P.tile([128, 128], F32) for i in range(4)]

        def load(ti):
            t0, tw = c.ttiles[ti]
            ub = ubs[ti % 2]
            P.dma("sp", ub[:, :, :tw], uT[:, t0:t0 + tw].rearrange("(kc p) t -> p kc t", p=128), writes=[ub], key=ub.dkey)

        def compute(ti):
            t0, tw = c.ttiles[ti]
            ub, go = ubs[ti % 2], gos[ti % 2]
            for sub in range(tw // 128):
                i = t0 // 128 + sub
                vt, vn, st = vts[i % 2], vns[i % 2], sts[i % 2]
                P.dma("sp", vt[:], vtok[i * 128:(i + 1) * 128, :], writes=[vt], key=vt.dkey)
                P.add("dve", lambda e, i=i, st=st: e.tensor_reduce(out=st[:, 0:1], in_=S1[:, i, :], axis=AX.X, op=ALU.add), reads=[S1], writes=[st])
                P.add("dve", lambda e, i=i, st=st: e.tensor_reduce(out=st[:, 1:2], in_=S2[:, i, :], axis=AX.X, op=ALU.add), reads=[S2], writes=[st])
                P.add("dve", lambda e, st=st: e.tensor_scalar(out=st[:, 2:3], in0=st[:, 0:1], scalar1=1.0 / D, scalar2=None, op0=ALU.mult), reads=[st], writes=[st])
                P.add("dve", lambda e, st=st: e.tensor_tensor(out=st[:, 3:4], in0=st[:, 2:3], in1=st[:, 2:3], op=ALU.mult), reads=[st], writes=[st])
                P.add("dve", lambda e, st=st: e.scalar_tensor_tensor(out=st[:, 4:5], in0=st[:, 1:2], scalar=1.0 / D, in1=st[:, 3:4], op0=ALU.mult, op1=ALU.subtract),
                      reads=[st], writes=[st])
                P.add("act", lambda e, st=st: e.activation(out=st[:, 5:6], in_=st[:, 4:5], func=AF.Sqrt, bias=1e-5, scale=1.0), reads=[st], writes=[st])
                P.add("dve", lambda e, st=st: e.reciprocal(out=st[:, 6:7], in_=st[:, 5:6]), reads=[st], writes=[st])
                P.add("dve", lambda e, st=st, vt=vt, vn=vn: e.tensor_scalar(out=vn[:], in0=vt[:], scalar1=st[:, 2:3], scalar2=st[:, 6:7], op0=ALU.subtract, op1=ALU.mult),
                      reads=[vt, st], writes=[vn])
                for fc in range(DC):
                    h = fc // CPH
                    pt = ps[(fc // 4) % 4]
                    tm = tmps[fc % 4]
                    P.add("pe", lambda e, fc=fc, h=h, pt=pt, vn=vn: e.matmul(pt[:, (fc % 4) * 128:(fc % 4 + 1) * 128], lhsT=vn[:, fc * 128:(fc + 1) * 128], rhs=WcT[:, h, :],
                                                                            start=True, stop=True),
                          reads=[vn, WcT], writes=[pt])
                    P.add("dve", lambda e, fc=fc, pt=pt, tm=tm: e.scalar_tensor_tensor(out=tm[:], in0=pt[:, (fc % 4) * 128:(fc % 4 + 1) * 128], scalar=lng[:, fc:fc + 1],
                                                                                    in1=Et[:, fc, :], op0=ALU.mult, op1=ALU.add),
                          reads=[pt, lng, Et], writes=[tm])
                    P.add("pool", lambda e, fc=fc, tm=tm, sub=sub: e.tensor_tensor(out=go[:, fc, sub * 128:(sub + 1) * 128], in0=tm[:], in1=ub[:, fc, sub * 128:(sub + 1) * 128], op=ALU.mult),
                          reads=[tm, ub], writes=[go])
            P.dma("sp", mT[:, t0:t0 + tw].rearrange("(c p) t -> p c t", p=128), go[:, :, :tw], reads=[go], key=go.dkey)

        pipeline(len(c.ttiles), load, compute)

    def stage_zero_slots():
        zi = P.tile([128, NSLOT // 128 * 2], I32)
        P.add("pool", lambda e: e.memset(zi[:], 0), writes=[zi])
        P.dma("sp", slot_tok.rearrange("(p f) o -> p (f o)", p=128), zi[:], reads=[zi], key="k_zs")

    def stage_experts(l):
        P.stage()

        wus = [P.tile([128, UPR], BF16, "w%d" % i) for i in range(2)]
        wdt = P.tile([128, DNR], BF16, "w2")
        sidxs = [P.tile([128, BS], I32, "i%d" % i) for i in range(2)]
        bups = [P.tile([128, 2 * FC], F32, "c%d" % i) for i in range(2)]
        bdr1 = P.tile([128, D], BF16, "b0")
        bdrs = [bdr1, bdr1]
        hgs = [P.tile([128, D], BF16, "h%d" % i) for i in range(2)]
        hbT = P.tile([128, DC, BLK], BF16)
        actT = P.tile([128, FC, BLK], BF16)
        yrs = [P.tile([128, D], BF16, "o%d" % i) for i in range(2)]
        g32, s32, l32 = (P.tile([128, BLK], F32) for _ in range(3))
        wup_v = m_wup.rearrange("l r (a b) -> (l r a) b", b=su)
        wdn_v = m_wdn.rearrange("l r (a b) -> (l r a) b", b=sd)
        bdn_v = m_bdn.rearrange("l r (a b) -> (l r a) b", b=sb)
        bup_v = m_bupT.rearrange("l r f -> (l r) f")
        wu_res = lambda wu: [(wu, a) for a in range(nsu)]
        wd_res = [(wdt, a) for a in range(nsd)]
        bd_res = lambda bdr: [(bdr, a) for a in range(nsb)]

        def load_up(b):
            wu, bup, bdr, sidx = wus[b % 2], bups[b % 2], bdrs[b % 2], sidxs[b % 2]
            P.dma("sp", sidx[:], slot_tok[b * BLK:(b + 1) * BLK, 0:1].rearrange("(s p) o -> p (s o)", p=128),
                  writes=[sidx], key=sidx.dkey, allow_slow_non_contiguous=True)
            for a in range(nsu):
                P.add("pool", lambda e, a=a: e.indirect_dma_start(
                    out=wu[:, a * su:(a + 1) * su], out_offset=None, in_=wup_v,
                    in_offset=bass.IndirectOffsetOnAxis(ap=WIU[:, b, a:a + 1], axis=0), bounds_check=P.reg(e, L * E * 128 * nsu - 1), oob_is_err=False),
                    reads=[WIU], writes=[(wu, a)], dma="%s_s%d" % (wu.dkey, a))
            P.add("pool", lambda e: e.indirect_dma_start(
                out=bup[:], out_offset=None, in_=bup_v,
                in_offset=bass.IndirectOffsetOnAxis(ap=WIDX[:, b:b + 1], axis=0), bounds_check=P.reg(e, L * E * 128 - 1), oob_is_err=False),
                reads=[WIDX], writes=[bup], dma=bup.dkey)

        def load_dn(b):
            bdr = bdr1
            for a in range(nsb):
                P.add("pool", lambda e, a=a: e.indirect_dma_start(
                    out=bdr[:, a * sb:(a + 1) * sb], out_offset=None, in_=bdn_v,
                    in_offset=bass.IndirectOffsetOnAxis(ap=BID[:, b, a:a + 1], axis=0), bounds_check=P.reg(e, L * E * nsb - 1), oob_is_err=False),
                    reads=[BID], writes=[(bdr, a)], dma="%s_s%d" % (bdr.dkey, a))
            for a in range(nsd):
                P.add("pool", lambda e, a=a: e.indirect_dma_start(
                    out=wdt[:, a * sd:(a + 1) * sd], out_offset=None, in_=wdn_v,
                    in_offset=bass.IndirectOffsetOnAxis(ap=WID[:, b, a:a + 1], axis=0), bounds_check=P.reg(e, L * E * 128 * nsd - 1), oob_is_err=False),
                    reads=[WID], writes=[(wdt, a)], dma="w2_s%d" % a)

        gcount = [0]
        ycount = [0]

        def compute(b):
            wu, bup, bdr, sidx = wus[b % 2], bups[b % 2], bdrs[b % 2], sidxs[b % 2]
            wuv = wu[:].rearrange("p (kc f) -> p kc f", kc=DC)
            wdv = wdt[:].rearrange("p (fc d) -> p fc d", fc=FC)
            for jj in range(BS):
                hg = hgs[gcount[0] % 2]
                gcount[0] += 1
                P.add("pool", lambda e, jj=jj, hg=hg: e.indirect_dma_start(
                    out=hg[:], out_offset=None, in_=hrows[:, :],
                    in_offset=bass.IndirectOffsetOnAxis(ap=sidx[:, jj:jj + 1], axis=0), bounds_check=P.reg(e, TC - 1), oob_is_err=False),
                    reads=[sidx], writes=[hg], dma=hg.dkey)
                for c8 in range(0, DC, 8):
                    pt = ps[4 + (c8 // 8) % 2]
                    ptb = pt[:].bitcast(BF16)
                    for cc in range(8):
                        ch = c8 + cc
                        P.add("pe", lambda e, ch=ch, cc=cc, ptb=ptb, hg=hg: e.transpose(out=ptb[:, cc * 128:(cc + 1) * 128], in_=hg[:, ch * 128:(ch + 1) * 128], identity=ident[:]),
                              reads=[hg, ident], writes=[pt])
                    src_v = ptb.rearrange("p (a b) -> p a b", a=8)
                    dst_v = hbT[:, c8:c8 + 8, jj * 128:(jj + 1) * 128]
                    if (c8 // 8) % 2 == 0:
                        P.add("act", lambda e, src_v=src_v, dst_v=dst_v: e.copy(out=dst_v, in_=src_v), reads=[pt], writes=[hbT])
                    else:
                        P.add("dve", lambda e, src_v=src_v, dst_v=dst_v: e.tensor_copy(out=dst_v, in_=src_v), reads=[pt], writes=[hbT])
            for fp in range(FC):
                pg, pl = ps[(2 * fp) % 4], ps[(2 * fp + 1) % 4]
                for (off, pt_) in ((0, pg), (FE, pl)):
                    for kc in range(DC):
                        P.add("pe", lambda e, kc=kc, off=off, pt_=pt_, fp=fp: e.matmul(pt_[:, :BLK], lhsT=wuv[:, kc, off + fp * 128:off + (fp + 1) * 128], rhs=hbT[:, kc, :],
                                                                                   start=(kc == 0), stop=(kc == DC - 1)),
                              reads=wu_res(wu) + [hbT], writes=[pt_])
                P.add("dve", lambda e, fp=fp, pg=pg: e.tensor_scalar(out=g32[:], in0=pg[:, :BLK], scalar1=bup[:, fp:fp + 1], scalar2=7.0, op0=ALU.add, op1=ALU.min),
                      reads=[pg, bup], writes=[g32])
                P.add("act", lambda e: e.activation(out=s32[:], in_=g32[:], func=AF.Sigmoid, scale=1.702), reads=[g32], writes=[s32])
                P.add("dve", lambda e, fp=fp, pl=pl: e.tensor_scalar(out=l32[:], in0=pl[:, :BLK], scalar1=bup[:, FC + fp:FC + fp + 1], scalar2=7.0, op0=ALU.add, op1=ALU.min),
                      reads=[pl, bup], writes=[l32])
                P.add("pool", lambda e: e.tensor_scalar(out=l32[:], in0=l32[:], scalar1=-7.0, scalar2=1.0, op0=ALU.max, op1=ALU.add), reads=[l32], writes=[l32])
                P.add("dve", lambda e: e.tensor_tensor(out=g32[:], in0=g32[:], in1=s32[:], op=ALU.mult), reads=[g32, s32], writes=[g32])
                P.add("dve", lambda e, fp=fp: e.tensor_tensor(out=actT[:, fp, :], in0=g32[:], in1=l32[:], op=ALU.mult), reads=[g32, l32], writes=[actT])
            for jj in range(BS):
                yr = yrs[ycount[0] % 2]
                ycount[0] += 1
                for nb in range(D // NBW):
                    pt = ps[nb % 4]
                    for kf in range(FC):
                        P.add("pe", lambda e, kf=kf, jj=jj, nb=nb, pt=pt: e.matmul(pt[:, :NBW], lhsT=actT[:, kf, jj * 128:(jj + 1) * 128], rhs=wdv[:, kf, nb * NBW:(nb + 1) * NBW],
                                                                                start=(kf == 0), stop=False),
                              reads=[actT] + wd_res, writes=[pt])
                    P.add("pe", lambda e, nb=nb, pt=pt: e.matmul(pt[:, :NBW], lhsT=ones_b[0:1, :], rhs=bdr[0:1, nb * NBW:(nb + 1) * NBW], start=False, stop=True),
                          reads=[ones_b] + bd_res(bdr), writes=[pt])
                    if nb % 2 == 0:
                        P.add("act", lambda e, nb=nb, pt=pt, yr=yr: e.copy(out=yr[:, nb * NBW:(nb + 1) * NBW], in_=pt[:, :NBW]), reads=[pt], writes=[yr])
                    else:
                        P.add("dve", lambda e, nb=nb, pt=pt, yr=yr: e.tensor_copy(out=yr[:, nb * NBW:(nb + 1) * NBW], in_=pt[:, :NBW]), reads=[pt], writes=[yr])
                r0 = b * BLK + jj * 128
                P.dma("sp", ypairs[r0:r0 + 128, :], yr[:], reads=[yr], key=yr.dkey)

        load_up(0)
        load_dn(0)
        if NB > 1:
            load_up(1)
        for b in range(NB):
            compute(b)
            if b + 1 < NB:
                load_dn(b + 1)
            if b + 2 < NB:
                load_up(b + 2)

    def stage_combine(l, last):
        P.stage()
        MG = P.tile([128, D], F32)
        mark = P.sp_off
        emit_mod(l, 1, [(2, MG)])
        P.stage(keep=mark)
        if last:
            gf = P.tile([128, D], F32, "m_g")
            P.dma("sp", gf[:], g_fin.partition_broadcast(128), writes=[gf], key="m_g")
        xts = [P.tile([128, D], F32, "x%d" % i) for i in range(2)]
        yk1 = [P.tile([128, D], BF16, "y0%d" % k) for k in range(4)]
        yks = [yk1, yk1]
        acc = P.tile([128, D], F32)
        acc2 = acc
        xo = xts
        junk = P.tile([128, D], BF16)
        sss = [P.tile([128, 4], F32) for i in range(2)]

        def load(i):
            P.dma("sp", xts[i % 2][:], xres[i * 128:(i + 1) * 128, :], writes=[xts[i % 2]], key=xts[i % 2].dkey)

        def gathers(i):
            for k in range(4):
                yk = yk1[k]
                P.add("pool", lambda e, i=i, k=k, yk=yk: e.indirect_dma_start(
                    out=yk[:], out_offset=None, in_=ypairs[:, :],
                    in_offset=bass.IndirectOffsetOnAxis(ap=IDX[:, i, k:k + 1], axis=0), bounds_check=P.reg(e, NSLOT + 127), oob_is_err=False),
                    reads=[IDX], writes=[yk], dma=yk.dkey)

        def compute(i):
            xt, ys, o, ss = xts[i % 2], yks[i % 2], xo[i % 2], sss[i % 2]
            P.add("dve", lambda e: e.tensor_scalar(out=acc[:], in0=ys[0][:], scalar1=GATE[:, i, 0:1], scalar2=None, op0=ALU.mult),
                  reads=[ys[0], GATE], writes=[acc])
            for k in range(1, 4):
                P.add("dve", lambda e, k=k: e.scalar_tensor_tensor(out=acc[:], in0=ys[k][:], scalar=GATE[:, i, k:k + 1], in1=acc[:], op0=ALU.mult, op1=ALU.add),
                      reads=[ys[k], GATE, acc], writes=[acc])
            if i + 1 < NT:
                gathers(i + 1)
            P.add("dve", lambda e: e.tensor_tensor(out=acc[:], in0=acc[:], in1=MG[:], op=ALU.mult), reads=[acc, MG], writes=[acc])
            P.add("pool", lambda e: e.tensor_tensor(out=o[:], in0=acc[:], in1=xt[:], op=ALU.add), reads=[acc, xt], writes=[o])
            if not last:
                P.dma("sp", xres[i * 128:(i + 1) * 128, :], o[:], reads=[o], key=o.dkey)
            else:
                if debug:
                    P.dma("sp", xres[i * 128:(i + 1) * 128, :], o[:], reads=[o], key=o.dkey)
                if i * 128 >= c.HALO:
                    P.add("act", lambda e: e.activation(out=junk[:], in_=o[:], func=AF.Square, accum_out=ss[:, 0:1]), reads=[o], writes=[junk, ss])
                    P.add("act", lambda e: e.activation(out=ss[:, 1:2], in_=ss[:, 0:1], func=AF.Sqrt, bias=1e-5, scale=1.0 / D), reads=[ss], writes=[ss])
                    P.add("dve", lambda e: e.reciprocal(out=ss[:, 2:3], in_=ss[:, 1:2]), reads=[ss], writes=[ss])
                    P.add("dve", lambda e: e.scalar_tensor_tensor(out=acc2[:], in0=o[:], scalar=ss[:, 2:3], in1=gf[:], op0=ALU.mult, op1=ALU.mult),
                          reads=[o, ss, gf], writes=[acc2])
                    r0 = i * 128 - c.HALO
                    P.dma("sp", out[r0:r0 + 128, :], acc2[:], reads=[acc2], key="k_out")

        gathers(0)
        pipeline(NT, load, compute)

    stages = []
    stages.append(("const", stage_const))
    for l in range(L):
        j = l // 2
        src = x_in if l == 0 else xres
        stages.append(("norm1_%d" % l, lambda l=l, src=src: stage_norm(l, 0, src, False)))
        if l % 2 == 0:
            stages.append(("pool_in_%d" % l, lambda j=j: stage_pool_in(j)))
            stages.append(("pool_grp_%d" % l, lambda j=j: stage_pool_group(j)))
            stages.append(("pool_out_%d" % l, lambda l=l, j=j, src=src: (stage_out_gemm(l, p_wout[j], src), stage_zero_slots())))
        else:
            stages.append(("gmlp_u_%d" % l, lambda j=j: stage_gmlp_u(j)))
            stages.append(("gmlp_v_%d" % l, lambda j=j: stage_gmlp_v(j)))
            stages.append(("gmlp_sp_%d" % l, lambda j=j: stage_gmlp_spatial(j)))
            stages.append(("gmlp_out_%d" % l, lambda l=l, j=j, src=src: (stage_out_gemm(l, g_wout[j], src), stage_zero_slots())))
        stages.append(("norm2_%d" % l, lambda l=l: stage_norm(l, 1, xres, True)))
        stages.append(("experts_%d" % l, lambda l=l: stage_experts(l)))
        stages.append(("combine_%d" % l, lambda l=l: stage_combine(l, l == L - 1)))
    P.usage = []
    for si, (name, fn) in enumerate(stages):
        if stop_after is not None and si > stop_after:
            break
        P.hwm = 0
        fn()
        P.usage.append((name, P.hwm))
    if not dry:
        P.emit()
    P.stage_names = [n for n, _ in stages]
    return nc, P, stack


def _colT(v):
    v = np.asarray(v)
    sh = v.shape
    return np.ascontiguousarray(v.reshape(sh[:-1] + (sh[-1] // 128, 128)).swapaxes(-1, -2))


def make_in_maps(cfg, inp):
    c = cfg
    f32 = np.float32
    x = np.asarray(inp["x"], dtype=f32)
    cvec = np.asarray(inp["c"], dtype=f32)
    shared = {
        "ident": np.eye(128, dtype=f32),
        "triu": np.triu(np.ones((128, 128), f32), 1),
        "tril": np.tril(np.ones((128, 128), f32), 0),
        "thr": (np.arange(c.NB, dtype=f32) * c.BLK)[None, :],
        "pcol": np.arange(128, dtype=f32)[:, None],
        "tokid": np.repeat((np.arange(c.NT, dtype=np.int32)[None, :] * 128 + np.arange(128, dtype=np.int32)[:, None]), 2, axis=1).astype(np.int32),
        "w_cond": np.asarray(inp["w_cond"], f32),
        "b_condT": _colT(np.asarray(inp["b_cond"], f32)),
        "w_mod": np.asarray(inp["w_mod"], f32),
        "b_mod": np.asarray(inp["b_mod"], f32),
        "g_norm_mix": np.asarray(inp["g_norm_mix"], f32),
        "g_norm_ffn": np.asarray(inp["g_norm_ffn"], f32),
        "pool_w_in": np.asarray(inp["pool_w_in"], f32),
        "pool_w_group": np.asarray(inp["pool_w_group"], f32),
        "pool_scaleT": _colT(np.asarray(inp["pool_scale"], f32)),
        "pool_w_out": np.asarray(inp["pool_w_out"], f32),
        "gmlp_w_in": np.asarray(inp["gmlp_w_in"], f32),
        "gmlp_b_inT": _colT(np.asarray(inp["gmlp_b_in"], f32)[:, :c.D]),
        "gmlp_b_in": np.asarray(inp["gmlp_b_in"], f32),
        "gmlp_ln_gT": _colT(np.asarray(inp["gmlp_ln_g"], f32)),
        "gmlp_ln_bT": _colT(np.asarray(inp["gmlp_ln_b"], f32)),
        "gmlp_w_s": np.asarray(inp["gmlp_w_s"], f32),
        "gmlp_b_s": np.asarray(inp["gmlp_b_s"], f32),
        "gmlp_w_out": np.asarray(inp["gmlp_w_out"], f32),
        "moe_w_router": np.asarray(inp["moe_w_router"], f32),
        "moe_b_router": np.asarray(inp["moe_b_router"], f32),
        "moe_w_upP": np.ascontiguousarray(np.asarray(inp["moe_w_up"], f32).reshape(c.DEPTH, c.E, c.DC, 128, 2 * c.FE).transpose(0, 1, 3, 2, 4)).reshape(c.DEPTH, c.E * 128, c.DC * 2 * c.FE),
        "moe_b_upT": _colT(np.asarray(inp["moe_b_up"], f32)).reshape(c.DEPTH, c.E * 128, 2 * c.FC),
        "moe_w_downP": np.ascontiguousarray(np.asarray(inp["moe_w_down"], f32).reshape(c.DEPTH, c.E, c.FC, 128, c.D).transpose(0, 1, 3, 2, 4)).reshape(c.DEPTH, c.E * 128, c.FC * c.D),
        "moe_b_down": np.asarray(inp["moe_b_down"], f32),
        "g_final": np.asarray(inp["g_final"], f32)[None, :],
    }
    maps = []
    wins = (2, 4, 8, 16)
    for core in range(c.BATCH * c.NQ):
        b, q = core // c.NQ, core % c.NQ
        start = q * c.SQ - c.HALO
        xc = np.zeros((c.TC, c.D), f32)
        lo = max(start, 0)
        xc[lo - start:, :] = x[b, lo:start + c.TC, :]
        pos = start + np.arange(c.TC)
        tokc = np.ones((5, c.TC), f32)
        tokc[0] = (pos >= 0).astype(f32)
        for gi, w in enumerate(wins):
            cnt = np.minimum(np.maximum(pos, 0) + 1, w).astype(f32)
            tokc[1 + gi] = 1.0 / cnt
        m = dict(shared)
        m["x"] = xc
        m["cT"] = _colT(cvec[b])
        m["tokc"] = tokc
        m["validT"] = np.ascontiguousarray(tokc[0].reshape(c.NT, 128).T)
        maps.append(m)
    return maps


_CACHE = {}


def kernel(**inputs):
    cfg = Cfg()
    if "nc" not in _CACHE:
        _CACHE["nc"] = build(cfg)
    nc, P, stack = _CACHE["nc"]
    maps = make_in_maps(cfg, inputs)
    ncores = cfg.BATCH * cfg.NQ
    res = run_bass_kernel_spmd(nc, maps, core_ids=list(range(ncores)))
    outp = np.empty((cfg.BATCH, cfg.SEQ, cfg.D), np.float32)
    for core in range(ncores):
        b, q = core // cfg.NQ, core % cfg.NQ
        outp[b, q * cfg.SQ:(q + 1) * cfg.SQ, :] = res.results[core]["out"]
    return outp
```

```python
import contextlib
import numpy as np
import concourse.bass as bass
import concourse.mybir as mybir
from concourse.bass_utils import run_bass_kernel_spmd

F32 = mybir.dt.float32
BF16 = mybir.dt.bfloat16
I32 = mybir.dt.int32
AF = mybir.ActivationFunctionType
ALU = mybir.AluOpType
AX = mybir.AxisListType
DSIZE = {F32: 4, BF16: 2, I32: 4}


class Cfg:
    def __init__(self, D=4096, SEQ=8192, BATCH=2, DEPTH=4, HEADS=8, FE=384, E=32, RANK=512,
                 BS=2, HALO=256, NQ=2, ARENA_KB=206):
        self.D, self.SEQ, self.BATCH, self.DEPTH, self.HEADS = D, SEQ, BATCH, DEPTH, HEADS
        self.BS = BS
        self.BLK = BS * 128
        self.FE, self.E, self.RANK, self.HALO, self.NQ = FE, E, RANK, HALO, NQ
        self.DC = D // 128
        self.RC = RANK // 128
        self.SQ = SEQ // NQ
        self.TC = self.SQ + HALO
        self.NT = self.TC // 128
        self.GW = D // 4
        self.HW = D // HEADS
        self.FC = FE // 128
        self.NB = (self.TC * 4 + self.BLK - 1) // self.BLK + E
        self.NSLOT = self.NB * self.BLK
        self.ARENA_KB = ARENA_KB
        self.NBW = min(512, D)
        tt = []
        t0 = 0
        while t0 < self.TC:
            tw = min(512, self.TC - t0)
            tt.append((t0, tw))
            t0 += tw
        self.ttiles = tt


class Op:
    __slots__ = ("eng", "fn", "deps", "inc", "ticket", "dma", "rw")

    def __init__(self, eng, fn, dma):
        self.eng, self.fn, self.dma = eng, fn, dma
        self.deps = []
        self.inc = dma is not None
        self.ticket = None


class Tile:
    def __init__(self, ap, dkey=None):
        self.ap = ap
        self.dkey = dkey

    def __getitem__(self, k):
        return self.ap[k]


class Prog:
    ENGS = ("pe", "act", "dve", "pool", "sp")
    BLK = {"pe": "tensor", "act": "scalar", "dve": "vector", "pool": "gpsimd", "sp": "sync"}
    LIMIT = 20000

    def __init__(self, nc, stack, arena_kb, dry=False):
        self.nc, self.stack = nc, stack
        self.ops = []
        self.lastw = {}
        self.readers = {}
        self.epoch = []
        self.epoch_done = {e: True for e in self.ENGS}
        self.last_inc = {e: None for e in self.ENGS}
        self.dma_last = {}
        self.regs = {}
        self.arena_elems = arena_kb * 1024 // 2
        if dry:
            self.arena_elems = 1024 * 512
            self.arena = nc.dram_tensor("arena_dry", [128, self.arena_elems], BF16).ap()
        else:
            self.arena = stack.enter_context(nc.sbuf_tensor("arena", [128, self.arena_elems], BF16))
        self.pbase = 0
        self.sp_off = 0
        self.psum = [Tile(stack.enter_context(nc.psum_tensor("ps%d" % i, [128, 512], F32))[:]) for i in range(8)]

    def _carve(self, off_bytes, shape, dtype):
        per = int(np.prod(shape[1:])) * DSIZE[dtype]
        a = self.arena[0:shape[0], off_bytes // 2:(off_bytes + per) // 2]
        if dtype != BF16:
            a = a.bitcast(dtype)
        if len(shape) == 3:
            a = a.rearrange("p (a b) -> p a b", a=shape[1])
        elif len(shape) == 4:
            a = a.rearrange("p (a b c) -> p a b c", a=shape[1], b=shape[2])
        return a, per

    def persist(self, shape, dtype, dkey=None):
        a, per = self._carve(self.pbase, shape, dtype)
        self.pbase += (per + 63) // 64 * 64
        self.sp_off = self.pbase
        return Tile(a, dkey)

    def tile(self, shape, dtype, dkey=None):
        per = int(np.prod(shape[1:])) * DSIZE[dtype]
        self.hwm = max(getattr(self, "hwm", 0), self.sp_off + per)
        if self.sp_off + per > self.arena_elems * 2:
            raise RuntimeError("arena overflow: need %d > %d" % (self.sp_off + per, self.arena_elems * 2))
        a, per = self._carve(self.sp_off, shape, dtype)
        self.sp_off += (per + 63) // 64 * 64
        return Tile(a, dkey)

    def stage(self, keep=None):
        self.epoch = [op for op in self.last_inc.values() if op is not None] + list(self.dma_last.values())
        for op in self.epoch:
            op.inc = True
        self.epoch_done = {e: False for e in self.ENGS}
        self.sp_off = self.pbase if keep is None else keep
        self.lastw.clear()
        self.readers.clear()

    def add(self, eng, fn, reads=(), writes=(), dma=None):
        if dma is not None:
            dma = "%s_%s" % (eng, dma)
        op = Op(eng, fn, dma)
        op.rw = (tuple(reads), tuple(writes))
        deps = {}

        def need(d, raw):
            if d is None or d is op:
                return
            if d.dma is None and dma is None and d.eng == eng and eng == "pe":
                return
            deps[id(d)] = d

        for r in reads:
            need(self.lastw.get(r), True)
        for w in writes:
            need(self.lastw.get(w), False)
            for rd in self.readers.get(w, ()):
                need(rd, False)
        if dma is not None:
            need(self.dma_last.get(dma), False)
            self.dma_last[dma] = op
        if not self.epoch_done[eng]:
            for d in self.epoch:
                if d is not op:
                    deps[id(d)] = d
            self.epoch_done[eng] = True
        for d in deps.values():
            d.inc = True
        op.deps = list(deps.values())
        for w in writes:
            self.lastw[w] = op
            self.readers[w] = []
        for r in reads:
            self.readers.setdefault(r, []).append(op)
        if dma is None:
            self.last_inc[eng] = op
        self.ops.append(op)
        return op

    def reg(self, e, val):
        if val not in self.regs:
            self.regs[val] = e.to_reg(val)
        return self.regs[val]

    def dma(self, q, out, in_, reads=(), writes=(), key=None, **kw):
        assert key is not None
        return self.add(q, lambda e: e.dma_start(out=out, in_=in_, **kw), reads, writes, dma=key)

    def emit(self):
        nc, stack = self.nc, self.stack
        self.stage()
        fin = self.epoch
        eng_sems = {e: [] for e in self.ENGS}
        cnt = {e: 0 for e in self.ENGS}
        dsem = {}
        nsem = [0]

        def newsem(name):
            nsem[0] += 1
            return stack.enter_context(nc.semaphore(name))

        for op in self.ops:
            if op.dma is not None:
                if op.dma not in dsem:
                    dsem[op.dma] = [newsem("d_%s" % op.dma), 0]
                s = dsem[op.dma]
                s[1] += 16
                op.ticket = (s[0], s[1])
            elif op.inc:
                if not eng_sems[op.eng] or cnt[op.eng] >= self.LIMIT:
                    eng_sems[op.eng].append(newsem("e_%s_%d" % (op.eng, len(eng_sems[op.eng]))))
                    cnt[op.eng] = 0
                cnt[op.eng] += 1
                op.ticket = (eng_sems[op.eng][-1], cnt[op.eng])
        self.n_sems = nsem[0]
        with nc.Block() as block:
            for eng in self.ENGS:
                ops_e = [op for op in self.ops if op.eng == eng]

                def body(e, ops_e=ops_e, eng=eng):
                    wm = {}

                    def wait_for(dlist):
                        needw = {}
                        for d in dlist:
                            sem, val = d.ticket
                            k = id(sem)
                            if wm.get(k, 0) < val and needw.get(k, (None, 0))[1] < val:
                                needw[k] = (sem, val)
                        for k, (sem, val) in needw.items():
                            e.wait_ge(sem, val)
                            wm[k] = val

                    for op in ops_e:
                        wait_for(op.deps)
                        ins = op.fn(e)
                        if op.ticket is not None:
                            ins.then_inc(op.ticket[0], 16 if op.dma is not None else 1)
                    if eng == "sp":
                        wait_for(fin)

                getattr(block, self.BLK[eng])(body)


def pipeline(n, load, compute, depth=2):
    for s in range(min(depth, n)):
        load(s)
    for s in range(n):
        compute(s)
        if s + depth < n:
            load(s + depth)


def build(cfg, debug=False, stop_after=None, dry=False):
    c = cfg
    D, DC, TC, NT, E, FE, FC, NSLOT, NB, BLK, BS = c.D, c.DC, c.TC, c.NT, c.E, c.FE, c.FC, c.NSLOT, c.NB, c.BLK, c.BS
    RC, NBW = c.RC, c.NBW
    L = c.DEPTH
    LP, LG = (L + 1) // 2, L // 2
    nc = bass.Bass("TRN2", target_bir_lowering=False)
    stack = contextlib.ExitStack()

    def din(name, shape, dt=F32):
        return nc.dram_tensor(name, list(shape), dt, kind="ExternalInput").ap()

    def dscr(name, shape, dt):
        return nc.dram_tensor(name, list(shape), dt, kind="ExternalOutput" if debug else "Internal").ap()

    x_in = din("x", [TC, D])
    cT_in = din("cT", [128, DC])
    tokc_in = din("tokc", [5, TC])
    ident_in = din("ident", [128, 128])
    triu_in = din("triu", [128, 128])
    tril_in = din("tril", [128, 128])
    thr_in = din("thr", [1, NB])
    pcol_in = din("pcol", [128, 1])
    tokid_in = din("tokid", [128, NT * 2], I32)
    validT_in = din("validT", [128, NT])
    w_cond = din("w_cond", [D, c.RANK])
    b_condT = din("b_condT", [128, RC])
    w_mod = din("w_mod", [L, c.RANK, 6 * D])
    b_mod = din("b_mod", [L, 6 * D])
    g_mix = din("g_norm_mix", [L, D])
    g_ffn = din("g_norm_ffn", [L, D])
    p_win = din("pool_w_in", [LP, D, D])
    p_wg = din("pool_w_group", [LP, 4, c.GW, c.GW])
    p_scT = din("pool_scaleT", [LP, 128, DC])
    p_wout = din("pool_w_out", [LP, D, D])
    g_win = din("gmlp_w_in", [LG, D, 2 * D])
    g_binT = din("gmlp_b_inT", [LG, 128, DC])
    g_bin = din("gmlp_b_in", [LG, 2 * D])
    g_lngT = din("gmlp_ln_gT", [LG, 128, DC])
    g_lnbT = din("gmlp_ln_bT", [LG, 128, DC])
    g_ws = din("gmlp_w_s", [LG, c.HEADS, 128, 128])
    g_bs = din("gmlp_b_s", [LG, c.HEADS, 128])
    g_wout = din("gmlp_w_out", [LG, D, D])
    m_wr = din("moe_w_router", [L, D, E])
    m_br = din("moe_b_router", [L, E])
    m_wup = din("moe_w_upP", [L, E * 128, DC * 2 * FE])
    m_bupT = din("moe_b_upT", [L, E * 128, 2 * FC])
    m_wdn = din("moe_w_downP", [L, E * 128, FC * D])
    m_bdn = din("moe_b_down", [L, E, D])
    g_fin = din("g_final", [1, D])
    out = nc.dram_tensor("out", [c.SQ, D], F32, kind="ExternalOutput").ap()

    xres = dscr("xres", [TC, D], F32)
    hT = dscr("hT", [D, TC], BF16)
    uT = dscr("uT", [D, TC], BF16)
    mT = dscr("mT", [D, TC], BF16)
    vtok = dscr("vtok", [TC, D], BF16)
    hrows = dscr("hrows", [TC, D], BF16)
    ypairs = dscr("ypairs", [NSLOT + 128, D], BF16)
    slot_tok = dscr("slot_tok", [NSLOT, 2], I32)

    P = Prog(nc, stack, c.ARENA_KB, dry)
    ps = P.psum

    ident = P.persist([128, 128], BF16, "k_id")
    identf = P.persist([128, 128], F32, "k_idf")
    triu = P.persist([128, 128], F32, "k_tu")
    tril = P.persist([128, 128], F32, "k_tl")
    ones_f = P.persist([128, 128], F32)
    ones_b = P.persist([128, 128], BF16)
    THR = P.persist([128, NB], F32, "k_eo")
    pcol = P.persist([128, 1], F32, "k_pc")
    def _seg(n):
        sg = min(n, 2048)
        while n % sg:
            sg -= 1
        return sg

    UPR, DNR = DC * 2 * FE, FC * D
    su, sd, sb = _seg(UPR), _seg(DNR), _seg(D)
    nsu, nsd, nsb = UPR // su, DNR // sd, D // sb
    WIDX = P.persist([128, NB], I32)
    WIU = P.persist([128, NB, nsu], I32)
    WID = P.persist([128, NB, nsd], I32)
    BID = P.persist([128, NB, nsb], I32)
    tokid = P.persist([128, NT * 2], I32, "k_ti")
    validT = P.persist([128, NT], F32, "k_va")
    condT = P.persist([128, RC], F32)
    condB = P.persist([128, RC, 128], F32)
    IDX = P.persist([128, NT, 4], I32)
    GATE = P.persist([128, NT, 4], F32)
    S1 = P.persist([128, NT, D // NBW], F32)
    S2 = P.persist([128, NT, D // NBW], F32)
    zrow = P.persist([128, 512], BF16)

    def stage_const():
        P.stage()
        P.dma("pool", ident[:], ident_in, writes=[ident], key="k_id")
        P.dma("sp", identf[:], ident_in, writes=[identf], key="k_idf")
        P.dma("sp", triu[:], triu_in, writes=[triu], key="k_tu")
        P.dma("sp", tril[:], tril_in, writes=[tril], key="k_tl")
        P.dma("sp", THR[:], thr_in.partition_broadcast(128), writes=[THR], key="k_eo")
        P.dma("sp", pcol[:], pcol_in, writes=[pcol], key="k_pc")
        P.dma("sp", tokid[:], tokid_in, writes=[tokid], key="k_ti")
        P.dma("sp", validT[:], validT_in, writes=[validT], key="k_va")
        P.add("pool", lambda e: e.memset(ones_f[:], 1.0), writes=[ones_f])
        P.add("pool", lambda e: e.memset(ones_b[:], 1.0), writes=[ones_b])
        P.add("pool", lambda e: e.memset(zrow[:], 0.0), writes=[zrow])
        for q in range(D // 512):
            P.dma("sp", ypairs[NSLOT:NSLOT + 128, q * 512:(q + 1) * 512], zrow[:], reads=[zrow], key="k_z")
        wc = P.tile([128, DC, c.RANK], BF16, "w0")
        ct = P.tile([128, DC], BF16, "x0")
        bc = P.tile([128, RC], F32, "x1")
        P.dma("pool", wc[:], w_cond.rearrange("(kc p) r -> p kc r", p=128), writes=[wc], key="w0")
        P.dma("pool", ct[:], cT_in, writes=[ct], key="x0")
        P.dma("sp", bc[:], b_condT, writes=[bc], key="x1")
        for rc in range(RC):
            for kc in range(DC):
                P.add("pe", lambda e, rc=rc, kc=kc: e.matmul(ps[6][:, rc:rc + 1], lhsT=wc[:, kc, rc * 128:(rc + 1) * 128],
                                                              rhs=ct[:, kc:kc + 1], start=(kc == 0), stop=(kc == DC - 1)),
                      reads=[wc, ct], writes=[ps[6]])
        for rc in range(RC):
            P.add("act", lambda e, rc=rc: e.activation(out=condT[:, rc:rc + 1], in_=ps[6][:, rc:rc + 1], func=AF.Silu,
                                                       bias=bc[:, rc:rc + 1], scale=1.0),
                  reads=[ps[6], bc], writes=[condT])
        for rc in range(RC):
            P.add("dve", lambda e, rc=rc: e.tensor_scalar(out=condB[:, rc, :], in0=ones_f[:], scalar1=condT[:, rc:rc + 1],
                                                          scalar2=None, op0=ALU.mult),
                  reads=[ones_f, condT], writes=[condB])

    def emit_mod(l, which, want):
        gsrc = (g_mix if which == 0 else g_ffn)
        gb = None
        if any(v == 1 for v, _ in want):
            gb = P.tile([128, D], F32, "m_g")
            P.dma("sp", gb[:], gsrc[l:l + 1, :].partition_broadcast(128), writes=[gb], key="m_g")
        wms = [P.tile([128, RC, 512], F32, "m_w%d" % i) for i in range(2)]
        bbs = [P.tile([128, 512], F32, "m_b%d" % i) for i in range(2)]
        tmp = P.tile([128, 512], F32)
        items = [(v, dst, nb) for v, dst in want for nb in range(D // NBW)]

        def load(s):
            v, dst, nb = items[s]
            col = (which * 3 + v) * D + nb * NBW
            wm, bb = wms[s % 2], bbs[s % 2]
            P.dma("sp", wm[:, :, :NBW], w_mod[l, :, col:col + NBW].rearrange("(rc p) n -> p rc n", p=128),
                  writes=[wm], key=wm.dkey)
            P.dma("sp", bb[:, :NBW], b_mod[l:l + 1, col:col + NBW].partition_broadcast(128), writes=[bb], key=bb.dkey)

        def compute(s):
            v, dst, nb = items[s]
            wm, bb = wms[s % 2], bbs[s % 2]
            pt = ps[s % 2]
            cs = slice(nb * NBW, (nb + 1) * NBW)
            for rc in range(RC):
                P.add("pe", lambda e, rc=rc: e.matmul(pt[:, :NBW], lhsT=condB[:, rc, :], rhs=wm[:, rc, :NBW],
                                                      start=(rc == 0), stop=(rc == RC - 1)),
                      reads=[condB, wm], writes=[pt])
            if v == 1:
                P.add("dve", lambda e: e.tensor_tensor(out=tmp[:, :NBW], in0=pt[:, :NBW], in1=bb[:, :NBW], op=ALU.add),
                      reads=[pt, bb], writes=[tmp])
                P.add("dve", lambda e: e.scalar_tensor_tensor(out=dst[:, cs], in0=tmp[:, :NBW], scalar=1.0, in1=gb[:, cs],
                                                               op0=ALU.add, op1=ALU.mult),
                      reads=[tmp, gb], writes=[dst])
            else:
                P.add("dve", lambda e: e.tensor_tensor(out=dst[:, cs], in0=pt[:, :NBW], in1=bb[:, :NBW], op=ALU.add),
                      reads=[pt, bb], writes=[dst])

        pipeline(len(items), load, compute)

    def stage_norm(l, which, src, moe):
        P.stage()
        MA = P.tile([128, D], F32)
        MB = P.tile([128, D], F32)
        mark = P.sp_off
        emit_mod(l, which, [(1, MA), (0, MB)])
        P.stage(keep=mark)
        xts = [P.tile([128, D], F32, "x%d" % i) for i in range(2)]
        t1 = P.tile([128, D], F32)
        hbs = [P.tile([128, D], BF16, "h%d" % i) for i in range(2)]
        htss = [P.tile([128, DC, 256], BF16, "o%d" % i) for i in range(2)]
        sss = [P.tile([128, 4], F32) for i in range(2)]
        if moe:
            wr = P.tile([128, DC, E], BF16, "w0")
            brr = P.tile([1, E], BF16, "w1")
            P.dma("pool", wr[:], m_wr[l].rearrange("(kc p) e -> p kc e", p=128), writes=[wr], key="w0")
            P.dma("pool", brr[:], m_br[l:l + 1, :], writes=[brr], key="w1")
            bases = [P.tile([1, E], F32) for i in range(2)]
            P.add("pool", lambda e: e.memset(bases[0][:], 0.0), writes=[bases[0]])
            P.add("pool", lambda e: e.memset(GATE[:], 0.0), writes=[GATE])
            sm = {n: P.tile([128, E], F32) for n in ("lg", "mk", "ex", "pm", "gts", "vl", "k1", "key", "oh", "gv", "jk")}
            t8 = P.tile([128, 8], F32)
            k8 = P.tile([128, 8], F32)
            RK = P.tile([128, NT, E], F32)
            MK = P.tile([128, NT, E], F32)
            GV = P.tile([128, NT, E], F32)
            s4 = {n: P.tile([128, 4], F32) for n in ("nm", "z4", "f4")}

        def load(i):
            P.dma("sp", xts[i % 2][:], src[i * 128:(i + 1) * 128, :], writes=[xts[i % 2]], key=xts[i % 2].dkey)

        def compute(i):
            xt, hb, ss = xts[i % 2], hbs[i % 2], sss[i % 2]
            g4, sub = i // 2, i % 2
            hts = htss[g4 % 2]
            P.add("act", lambda e: e.activation(out=hb[:], in_=xt[:], func=AF.Square, accum_out=ss[:, 0:1]),
                  reads=[xt], writes=[hb, ss])
            P.add("act", lambda e: e.activation(out=ss[:, 1:2], in_=ss[:, 0:1], func=AF.Sqrt, bias=1e-5, scale=1.0 / D),
                  reads=[ss], writes=[ss])
            P.add("dve", lambda e: e.reciprocal(out=ss[:, 2:3], in_=ss[:, 1:2]), reads=[ss], writes=[ss])
            P.add("dve", lambda e: e.scalar_tensor_tensor(out=t1[:], in0=xt[:], scalar=ss[:, 2:3], in1=MA[:],
                                                           op0=ALU.mult, op1=ALU.mult),
                  reads=[xt, ss, MA], writes=[t1])
            P.add("pool", lambda e: e.tensor_tensor(out=hb[:], in0=t1[:], in1=MB[:], op=ALU.add),
                  reads=[t1, MB], writes=[hb])
            if moe:
                P.dma("sp", hrows[i * 128:(i + 1) * 128, :], hb[:], reads=[hb], key=hb.dkey)
            for c8 in range(0, DC, 8):
                pt = ps[4 + (c8 // 8) % 2]
                ptb = pt[:].bitcast(BF16)
                for cc in range(8):
                    ch = c8 + cc
                    P.add("pe", lambda e, ch=ch, cc=cc, ptb=ptb: e.transpose(out=ptb[:, cc * 128:(cc + 1) * 128],
                                                                              in_=hb[:, ch * 128:(ch + 1) * 128],
                                                                              identity=ident[:]),
                          reads=[hb, ident], writes=[pt])
                eng = "act" if (c8 // 8) % 2 == 0 else "dve"
                src_v = ptb.rearrange("p (a b) -> p a b", a=8)
                dst_v = hts[:, c8:c8 + 8, sub * 128:(sub + 1) * 128]
                if eng == "act":
                    P.add("act", lambda e, src_v=src_v, dst_v=dst_v: e.copy(out=dst_v, in_=src_v), reads=[pt], writes=[hts])
                else:
                    P.add("dve", lambda e, src_v=src_v, dst_v=dst_v: e.tensor_copy(out=dst_v, in_=src_v), reads=[pt], writes=[hts])
            if moe:
                emit_router(i, hts, sub)
            if sub == 1 or i == NT - 1:
                t0 = g4 * 256
                tw = (sub + 1) * 128
                if not moe:
                    P.dma("sp", hT[:, t0:t0 + tw].rearrange("(c p) t -> p c t", p=128), hts[:, :, :tw], reads=[hts], key=hts.dkey)

        def emit_router(i, hts, sub):
            pr, pk, pb = ps[6], ps[7], ps[6]
            lg, mk, ex, pm, gts, vl, k1, key, oh, gv, jk = (sm[n] for n in ("lg", "mk", "ex", "pm", "gts", "vl", "k1", "key", "oh", "gv", "jk"))
            nm, z4, f4 = s4["nm"], s4["z4"], s4["f4"]
            b_old, b_new = bases[i % 2], bases[(i + 1) % 2]
            for kc in range(DC):
                P.add("pe", lambda e, kc=kc: e.matmul(pr[:, 0:E], lhsT=hts[:, kc, sub * 128:(sub + 1) * 128], rhs=wr[:, kc, :],
                                                      start=(kc == 0), stop=False),
                      reads=[hts, wr], writes=[pr])
            P.add("pe", lambda e: e.matmul(pr[:, 0:E], lhsT=ones_b[0:1, :], rhs=brr[0:1, :], start=False, stop=True),
                  reads=[ones_b, brr], writes=[pr])
            P.add("dve", lambda e: e.tensor_copy(out=lg[:], in_=pr[:, 0:E]), reads=[pr], writes=[lg])
            P.add("dve", lambda e: e.max(out=t8[:], in_=lg[:]), reads=[lg], writes=[t8])
            P.add("dve", lambda e: e.tensor_scalar(out=mk[:], in0=lg[:], scalar1=t8[:, 3:4], scalar2=validT[:, i:i + 1], op0=ALU.is_ge, op1=ALU.mult),
                  reads=[lg, t8, validT], writes=[mk])
            P.add("dve", lambda e: e.tensor_scalar(out=nm[:, 0:1], in0=t8[:, 0:1], scalar1=-1.0, scalar2=None, op0=ALU.mult),
                  reads=[t8], writes=[nm])
            P.add("act", lambda e: e.activation(out=ex[:], in_=lg[:], func=AF.Exp, bias=nm[:, 0:1], scale=1.0),
                  reads=[lg, nm], writes=[ex])
            P.add("dve", lambda e: e.scalar_tensor_tensor(out=pm[:], in0=ex[:], scalar=1.0, in1=mk[:], op0=ALU.mult, op1=ALU.mult,
                                                           accum_out=nm[:, 1:2]),
                  reads=[ex, mk], writes=[pm, nm])
            P.add("dve", lambda e: e.tensor_scalar(out=nm[:, 3:4], in0=nm[:, 1:2], scalar1=1e-30, scalar2=None, op0=ALU.add), reads=[nm], writes=[nm])
            P.add("dve", lambda e: e.reciprocal(out=nm[:, 2:3], in_=nm[:, 3:4]), reads=[nm], writes=[nm])
            P.add("dve", lambda e: e.tensor_scalar(out=gts[:], in0=pm[:], scalar1=nm[:, 2:3], scalar2=None, op0=ALU.mult),
                  reads=[pm, nm], writes=[gts])
            P.add("pe", lambda e: e.matmul(pk[:, 0:E], lhsT=triu[:], rhs=mk[:], start=True, stop=False), reads=[triu, mk], writes=[pk])
            P.add("pe", lambda e: e.matmul(pk[:, 0:E], lhsT=ones_f[0:1, :], rhs=b_old[0:1, :], start=False, stop=True),
                  reads=[ones_f, b_old], writes=[pk])
            P.add("pe", lambda e: e.matmul(pb[0:1, 64:64 + E], lhsT=ones_f[:, 0:1], rhs=mk[:], start=True, stop=False),
                  reads=[ones_f, mk, lg], writes=[pb])
            P.add("pe", lambda e: e.matmul(pb[0:1, 64:64 + E], lhsT=ones_f[0:1, 0:1], rhs=b_old[0:1, :], start=False, stop=True),
                  reads=[ones_f, b_old], writes=[pb])
            P.add("dve", lambda e: e.tensor_copy(out=b_new[:], in_=pb[0:1, 64:64 + E]), reads=[pb], writes=[b_new])
            P.add("dve", lambda e: e.tensor_copy(out=RK[:, i, :], in_=pk[:, 0:E]), reads=[pk], writes=[RK])
            P.add("act", lambda e: e.copy(out=MK[:, i, :], in_=mk[:]), reads=[mk], writes=[MK])
            P.add("act", lambda e: e.copy(out=GV[:, i, :], in_=gts[:]), reads=[gts], writes=[GV])

        def emit_dispatch():
            cnt = bases[NT % 2]
            r1 = P.tile([1, 2 * E], F32)
            r1i = P.tile([1, 2 * E], I32)
            sc = [P.tile([1, 2 * E], F32) for _ in range(2)]
            PB = P.tile([128, 2 * E], F32)
            EB = P.tile([128, NB], F32)
            EB2 = P.tile([128, NB], F32)
            pq = ps[7]
            P.add("dve", lambda e: e.tensor_scalar(out=r1[:, 0:E], in0=cnt[:], scalar1=BLK / 2 - 0.5, scalar2=1.0 / BLK, op0=ALU.add, op1=ALU.mult),
                  reads=[cnt], writes=[r1])
            P.add("dve", lambda e: e.tensor_copy(out=r1i[:, 0:E], in_=r1[:, 0:E]), reads=[r1], writes=[r1i])
            P.add("dve", lambda e: e.tensor_copy(out=r1[:, 0:E], in_=r1i[:, 0:E]), reads=[r1i], writes=[r1])
            P.add("dve", lambda e: e.memset(sc[0][:], 0.0), writes=[sc[0]])
            P.add("dve", lambda e: e.memset(sc[1][:], 0.0), writes=[sc[1]])
            P.add("dve", lambda e: e.tensor_scalar(out=sc[0][:, E:2 * E], in0=r1[:, 0:E], scalar1=float(BLK), scalar2=None, op0=ALU.mult),
                  reads=[r1], writes=[sc[0]])
            P.add("dve", lambda e: e.tensor_copy(out=r1[:, E:2 * E], in_=sc[0][:, E:2 * E]), reads=[sc[0]], writes=[r1])
            cur = 0
            sh = 1
            while sh < E:
                a, b = sc[cur], sc[1 - cur]
                P.add("dve", lambda e, a=a, b=b, sh=sh: e.tensor_tensor(out=b[:, E:2 * E], in0=a[:, E:2 * E], in1=a[:, E - sh:2 * E - sh], op=ALU.add),
                      reads=[a], writes=[b])
                cur = 1 - cur
                sh *= 2
            pend = sc[cur]
            P.add("dve", lambda e: e.tensor_tensor(out=r1[:, 0:E], in0=pend[:, E:2 * E], in1=r1[:, E:2 * E], op=ALU.subtract), reads=[pend, r1], writes=[r1])
            P.add("dve", lambda e: e.tensor_copy(out=r1[:, E:2 * E], in_=pend[:, E:2 * E]), reads=[pend], writes=[r1])
            P.add("pe", lambda e: e.matmul(pq[:, 0:2 * E], lhsT=ones_f[0:1, :], rhs=r1[0:1, :], start=True, stop=True), reads=[ones_f, r1], writes=[pq])
            P.add("dve", lambda e: e.tensor_copy(out=PB[:], in_=pq[:, 0:2 * E]), reads=[pq], writes=[PB])
            P.add("dve", lambda e: e.memset(EB[:], 0.0), writes=[EB])
            cur_e, oth_e = EB, EB2
            for ex_ in range(E):
                P.add("dve", lambda e, ex_=ex_, cur_e=cur_e, oth_e=oth_e: e.scalar_tensor_tensor(
                    out=oth_e[:], in0=THR[:], scalar=PB[:, E + ex_:E + ex_ + 1], in1=cur_e[:], op0=ALU.is_ge, op1=ALU.add),
                    reads=[THR, PB, cur_e], writes=[oth_e])
                cur_e, oth_e = oth_e, cur_e
            P.add("dve", lambda e, cur_e=cur_e, oth_e=oth_e: e.tensor_scalar(out=oth_e[:], in0=cur_e[:], scalar1=float(E - 1), scalar2=None, op0=ALU.min),
                  reads=[cur_e], writes=[oth_e])
            ebf = oth_e
            P.add("dve", lambda e, cur_e=cur_e: e.tensor_scalar(out=cur_e[:], in0=ebf[:], scalar1=float(l * E), scalar2=None, op0=ALU.add),
                  reads=[ebf], writes=[cur_e])
            tmpf = P.tile([128, NB], F32)
            for a in range(nsb):
                P.add("dve", lambda e, a=a, cur_e=cur_e: e.tensor_scalar(out=tmpf[:], in0=cur_e[:], scalar1=float(nsb), scalar2=float(a), op0=ALU.mult, op1=ALU.add),
                      reads=[cur_e], writes=[tmpf])
                P.add("dve", lambda e, a=a: e.tensor_copy(out=BID[:, :, a], in_=tmpf[:]), reads=[tmpf], writes=[BID])
            P.add("dve", lambda e, cur_e=cur_e: e.tensor_scalar(out=ebf[:], in0=cur_e[:], scalar1=128.0, scalar2=pcol[:, 0:1], op0=ALU.mult, op1=ALU.add),
                  reads=[cur_e, pcol], writes=[ebf])
            P.add("dve", lambda e: e.tensor_copy(out=WIDX[:], in_=ebf[:]), reads=[ebf], writes=[WIDX])
            sk = {2: P.tile([128, NB], F32), 1: P.tile([128, NB], F32)}
            for lag in (2, 1):
                P.add("dve", lambda e, lag=lag: e.memset(sk[lag][:], 0.0), writes=[sk[lag]])
                if NB > lag:
                    P.add("dve", lambda e, lag=lag, cur_e=cur_e: e.tensor_tensor(out=sk[lag][:, lag:NB], in0=cur_e[:, lag:NB], in1=cur_e[:, 0:NB - lag], op=ALU.is_equal),
                          reads=[cur_e], writes=[sk[lag]])
            for (ns_, dst_, lag) in ((nsu, WIU, 2), (nsd, WID, 1)):
                for a in range(ns_):
                    P.add("dve", lambda e, a=a, ns_=ns_: e.tensor_scalar(out=tmpf[:], in0=ebf[:], scalar1=float(ns_), scalar2=float(a), op0=ALU.mult, op1=ALU.add),
                          reads=[ebf], writes=[tmpf])
                    P.add("dve", lambda e, lag=lag, ns_=ns_: e.scalar_tensor_tensor(out=tmpf[:], in0=sk[lag][:], scalar=float(L * E * 128 * ns_), in1=tmpf[:], op0=ALU.mult, op1=ALU.add),
                          reads=[tmpf, sk[lag]], writes=[tmpf])
                    P.add("dve", lambda e, a=a, dst_=dst_: e.tensor_copy(out=dst_[:, :, a], in_=tmpf[:]), reads=[tmpf], writes=[dst_])
            key, oh, jk, k1 = sm["key"], sm["oh"], sm["jk"], sm["k1"]
            z4, f4 = s4["z4"], s4["f4"]
            for i in range(NT):
                P.add("dve", lambda e, i=i: e.scalar_tensor_tensor(out=k1[:], in0=RK[:, i, :], scalar=1.0, in1=PB[:, 0:E], op0=ALU.add, op1=ALU.add),
                      reads=[RK, PB], writes=[k1])
                P.add("dve", lambda e, i=i: e.tensor_tensor(out=key[:], in0=k1[:], in1=MK[:, i, :], op=ALU.mult), reads=[k1, MK], writes=[key])
                P.add("dve", lambda e: e.max(out=k8[:], in_=key[:]), reads=[key], writes=[k8])
                P.add("dve", lambda e: e.tensor_scalar(out=z4[:], in0=k8[:, 0:4], scalar1=0.0, scalar2=float(NSLOT + 1),
                                                       op0=ALU.is_equal, op1=ALU.mult), reads=[k8], writes=[z4])
                P.add("dve", lambda e: e.scalar_tensor_tensor(out=f4[:], in0=k8[:, 0:4], scalar=-1.0, in1=z4[:], op0=ALU.add, op1=ALU.add),
                      reads=[k8, z4], writes=[f4])
                P.add("dve", lambda e, i=i: e.tensor_copy(out=IDX[:, i, :], in_=f4[:]), reads=[f4], writes=[IDX])
                for k in range(4):
                    P.add("dve", lambda e, k=k: e.tensor_scalar(out=oh[:], in0=key[:], scalar1=k8[:, k:k + 1], scalar2=None, op0=ALU.is_equal),
                          reads=[key, k8], writes=[oh])
                    P.add("dve", lambda e, k=k, i=i: e.scalar_tensor_tensor(out=jk[:], in0=oh[:], scalar=1.0, in1=GV[:, i, :], op0=ALU.mult, op1=ALU.mult,
                                                                            accum_out=GATE[:, i, k:k + 1]),
                          reads=[oh, GV], writes=[jk, GATE])
                for k in range(4):
                    P.add("pool", lambda e, k=k, i=i: e.indirect_dma_start(
                        out=slot_tok[:, :], out_offset=bass.IndirectOffsetOnAxis(ap=IDX[:, i, k:k + 1], axis=0),
                        in_=tokid[:, 2 * i:2 * i + 2], in_offset=None, bounds_check=P.reg(e, NSLOT - 1), oob_is_err=False),
                        reads=[IDX, tokid], dma="k_sc%d" % k)

        pipeline(NT, load, compute)
        if moe:
            emit_dispatch()

    def gemm_fm(W, X, KC, N, epi, post, extra_load=None):
        nbw = min(512, N)
        mch = nbw // 128
        nnb = N // nbw
        wbs = [P.tile([128, KC, nbw], BF16, "w%d" % i) for i in range(2)]
        xbs = [P.tile([128, KC, 512], BF16, "x%d" % i) for i in range(2)]
        items = [(nbi, ti) for nbi in range(nnb) for ti in range(len(c.ttiles))]

        def loadw(nbi):
            wb = wbs[nbi % 2]
            P.dma("pool", wb[:], W[:, nbi * nbw:(nbi + 1) * nbw].rearrange("(kc p) n -> p kc n", p=128), writes=[wb], key=wb.dkey)

        def load(s):
            nbi, ti = items[s]
            t0, tw = c.ttiles[ti]
            xb = xbs[s % 2]
            P.dma("sp", xb[:, :, :tw], X[:, t0:t0 + tw].rearrange("(kc p) t -> p kc t", p=128), writes=[xb], key=xb.dkey)
            if extra_load is not None:
                extra_load(s, nbi, ti, t0, tw)

        def compute(s):
            nbi, ti = items[s]
            t0, tw = c.ttiles[ti]
            wb, xb = wbs[nbi % 2], xbs[s % 2]
            for m in range(mch):
                pt = ps[(s * mch + m) % 4]
                for kc in range(KC):
                    P.add("pe", lambda e, kc=kc, m=m, pt=pt: e.matmul(pt[:, :tw], lhsT=wb[:, kc, m * 128:(m + 1) * 128], rhs=xb[:, kc, :tw],
                                                                   start=(kc == 0), stop=(kc == KC - 1)),
                          reads=[wb, xb], writes=[pt])
                epi(s, nbi, m, ti, t0, tw, pt)
            post(s, nbi, ti, t0, tw)
            if ti == len(c.ttiles) - 1 and nbi + 2 < nnb:
                loadw(nbi + 2)

        for nbi in range(min(2, nnb)):
            loadw(nbi)
        pipeline(len(items), load, compute)

    def gemm_tm(W, X, KC, N, bias, epi, post, extra_load=None):
        nbw = min(512, N)
        nnb = N // nbw
        wbs = [P.tile([128, KC, nbw], BF16, "w%d" % i) for i in range(2)]
        xbs = [P.tile([128, KC, 512], BF16, "x%d" % i) for i in range(2)]
        brs = [P.tile([1, nbw], BF16, "b%d" % i) for i in range(2)] if bias is not None else None
        items = [(nbi, ti) for nbi in range(nnb) for ti in range(len(c.ttiles))]

        def loadw(nbi):
            wb = wbs[nbi % 2]
            P.dma("pool", wb[:], W[:, nbi * nbw:(nbi + 1) * nbw].rearrange("(kc p) n -> p kc n", p=128), writes=[wb], key=wb.dkey)
            if bias is not None:
                br = brs[nbi % 2]
                P.dma("pool", br[:], bias[:, nbi * nbw:(nbi + 1) * nbw], writes=[br], key=br.dkey)

        def load(s):
            nbi, ti = items[s]
            t0, tw = c.ttiles[ti]
            xb = xbs[s % 2]
            P.dma("sp", xb[:, :, :tw], X[:, t0:t0 + tw].rearrange("(kc p) t -> p kc t", p=128), writes=[xb], key=xb.dkey)
            if extra_load is not None:
                extra_load(s, nbi, ti, t0, tw)

        def compute(s):
            nbi, ti = items[s]
            t0, tw = c.ttiles[ti]
            wb, xb = wbs[nbi % 2], xbs[s % 2]
            for sub in range(tw // 128):
                pt = ps[(s * 4 + sub) % 4]
                for kc in range(KC):
                    P.add("pe", lambda e, kc=kc, sub=sub, pt=pt: e.matmul(pt[:, :nbw], lhsT=xb[:, kc, sub * 128:(sub + 1) * 128], rhs=wb[:, kc, :],
                                                                       start=(kc == 0), stop=(kc == KC - 1 and bias is None)),
                          reads=[wb, xb], writes=[pt])
                if bias is not None:
                    br = brs[nbi % 2]
                    P.add("pe", lambda e, pt=pt, br=br: e.matmul(pt[:, :nbw], lhsT=ones_b[0:1, :], rhs=br[0:1, :], start=False, stop=True),
                          reads=[ones_b, br], writes=[pt])
                epi(s, nbi, ti, sub, t0 // 128 + sub, pt, nbw)
            post(s, nbi, ti, t0, tw, nbw)
            if ti == len(c.ttiles) - 1 and nbi + 2 < nnb:
                loadw(nbi + 2)

        for nbi in range(min(2, nnb)):
            loadw(nbi)
        pipeline(len(items), load, compute)

    def stage_out_gemm(l, W, xsrc):
        P.stage()
        MG = P.tile([128, D], F32)
        mark = P.sp_off
        emit_mod(l, 0, [(2, MG)])
        P.stage(keep=mark)
        xfs = [P.tile([128, 4, 512], F32, "r%d" % i) for i in range(2)]
        xos = xfs
        tmp = [P.tile([128, 512], F32) for i in range(2)]

        def extra_load(s, nbi, ti, t0, tw):
            xf = xfs[s % 2]
            P.dma("sp", xf[:, :tw // 128, :NBW], xsrc[t0:t0 + tw, nbi * NBW:(nbi + 1) * NBW].rearrange("(a p) n -> p a n", p=128),
                  writes=[xf], key=xf.dkey)

        def epi(s, nbi, ti, sub, itile, pt, nbw):
            xf, xo, tm = xfs[s % 2], xos[s % 2], tmp[sub % 2]
            P.add("dve", lambda e: e.tensor_tensor(out=tm[:, :nbw], in0=pt[:, :nbw], in1=MG[:, nbi * nbw:(nbi + 1) * nbw], op=ALU.mult),
                  reads=[pt, MG], writes=[tm])
            P.add("pool", lambda e: e.tensor_tensor(out=xo[:, sub, :nbw], in0=tm[:, :nbw], in1=xf[:, sub, :nbw], op=ALU.add),
                  reads=[tm, xf], writes=[xo])

        def post(s, nbi, ti, t0, tw, nbw):
            xo = xos[s % 2]
            P.dma("sp", xres[t0:t0 + tw, nbi * nbw:(nbi + 1) * nbw].rearrange("(a p) n -> p a n", p=128), xo[:, :tw // 128, :nbw],
                  reads=[xo], key=xo.dkey)

        gemm_tm(W, mT, DC, D, None, epi, post, extra_load)

    def stage_pool_in(j):
        P.stage()
        GC = DC // 4
        tcs = [P.tile([128, 5, 512], F32, "c%d" % i) for i in range(2)]
        obs = [P.tile([128, 4, 512], BF16, "o%d" % i) for i in range(2)]
        ues = [[P.tile([128, 16 + 512], F32) for par in range(2)] for m in range(4)]
        sA = [P.tile([128, 16 + 512], F32) for i in range(2)]
        sB = [P.tile([128, 16 + 512], F32) for i in range(2)]
        for m in range(4):
            P.add("pool", lambda e, m=m: e.memset(ues[m][0][:, 0:16], 0.0), writes=[ues[m][0]])

        def extra_load(s, nbi, ti, t0, tw):
            tcn = tcs[s % 2]
            P.dma("sp", tcn[:, :, :tw], tokc_in[:, t0:t0 + tw].partition_broadcast(128), writes=[tcn], key=tcn.dkey)

        def epi(s, nbi, m, ti, t0, tw, pt):
            fc = nbi * (NBW // 128) + m
            g = fc // GC
            tcn, ob = tcs[s % 2], obs[s % 2]
            ue, uen = ues[m][ti % 2], ues[m][(ti + 1) % 2]
            a, b = sA[m % 2], sB[m % 2]
            Wd = 16 + tw
            if ti == 0 and nbi > 0:
                P.add("pool", lambda e: e.memset(ue[:, 0:16], 0.0), writes=[ue])
            P.add("dve", lambda e: e.tensor_tensor(out=ue[:, 16:Wd], in0=pt[:, :tw], in1=tcn[:, 0, :tw], op=ALU.mult),
                  reads=[pt, tcn], writes=[ue])
            if ti + 1 < len(c.ttiles):
                P.add("act", lambda e: e.copy(out=uen[:, 0:16], in_=ue[:, tw:tw + 16]), reads=[ue], writes=[uen])
            bufs = [ue, a, b, a, b]
            for st in range(g + 1):
                sh = 1 << st
                lo = 2 * sh - 1
                src_t, dst_t = bufs[st], bufs[st + 1]
                P.add("pool", lambda e, src_t=src_t, dst_t=dst_t, sh=sh, lo=lo: e.tensor_tensor(
                    out=dst_t[:, lo:Wd], in0=src_t[:, lo:Wd], in1=src_t[:, lo - sh:Wd - sh], op=ALU.add),
                    reads=[src_t], writes=[dst_t])
            res = bufs[g + 1]
            oth = b if res is a else a
            P.add("dve", lambda e: e.tensor_tensor(out=oth[:, 16:Wd], in0=res[:, 16:Wd], in1=tcn[:, 1 + g, :tw], op=ALU.mult),
                  reads=[res, tcn], writes=[oth])
            P.add("dve", lambda e: e.tensor_tensor(out=ob[:, m, :tw], in0=oth[:, 16:Wd], in1=ue[:, 16:Wd], op=ALU.subtract),
                  reads=[oth, ue], writes=[ob])

        def post(s, nbi, ti, t0, tw):
            ob = obs[s % 2]
            mch = NBW // 128
            P.dma("sp", uT[nbi * NBW:(nbi + 1) * NBW, t0:t0 + tw].rearrange("(m p) t -> p m t", p=128), ob[:, :mch, :tw],
                  reads=[ob], key=ob.dkey)

        gemm_fm(p_win[j], hT, DC, D, epi, post, extra_load)

    def stage_pool_group(j):
        P.stage()
        GW = c.GW
        KCg = GW // 128
        psc = P.tile([128, DC], F32, "c0")
        P.dma("sp", psc[:], p_scT[j], writes=[psc], key="c0")
        obs = [P.tile([128, 4, 512], BF16, "o%d" % i) for i in range(2)]
        for g in range(4):
            nbw = min(512, GW)

            def epi(s, nbi, m, ti, t0, tw, pt, g=g, nbw=nbw):
                fc = (g * GW + nbi * nbw) // 128 + m
                ob = obs[s % 2]
                if m % 2 == 0:
                    P.add("dve", lambda e: e.tensor_scalar(out=ob[:, m, :tw], in0=pt[:, :tw], scalar1=psc[:, fc:fc + 1], scalar2=None, op0=ALU.mult),
                          reads=[pt, psc], writes=[ob])
                else:
                    P.add("act", lambda e: e.activation(out=ob[:, m, :tw], in_=pt[:, :tw], func=AF.Copy, scale=psc[:, fc:fc + 1]),
                          reads=[pt, psc], writes=[ob])

            def post(s, nbi, ti, t0, tw, g=g, nbw=nbw):
                ob = obs[s % 2]
                r0 = g * GW + nbi * nbw
                P.dma("sp", mT[r0:r0 + nbw, t0:t0 + tw].rearrange("(m p) t -> p m t", p=128), ob[:, :nbw // 128, :tw],
                      reads=[ob], key=ob.dkey)

            gemm_fm(p_wg[j, g], uT[g * GW:(g + 1) * GW, :], KCg, GW, epi, post)

    def stage_gmlp_u(j):
        P.stage()
        bT = P.tile([128, DC], F32, "c0")
        P.dma("sp", bT[:], g_binT[j], writes=[bT], key="c0")
        obs = [P.tile([128, 4, 512], BF16, "o%d" % i) for i in range(2)]

        def epi(s, nbi, m, ti, t0, tw, pt):
            fc = nbi * (NBW // 128) + m
            ob = obs[s % 2]
            P.add("act", lambda e: e.activation(out=ob[:, m, :tw], in_=pt[:, :tw], func=AF.Gelu, bias=bT[:, fc:fc + 1], scale=1.0),
                  reads=[pt, bT], writes=[ob])

        def post(s, nbi, ti, t0, tw):
            ob = obs[s % 2]
            P.dma("sp", uT[nbi * NBW:(nbi + 1) * NBW, t0:t0 + tw].rearrange("(m p) t -> p m t", p=128), ob[:, :NBW // 128, :tw],
                  reads=[ob], key=ob.dkey)

        gemm_fm(g_win[j, :, 0:D], hT, DC, D, epi, post)

    def stage_gmlp_v(j):
        P.stage()
        P.add("pool", lambda e: e.memset(S1[:], 0.0), writes=[S1])
        P.add("pool", lambda e: e.memset(S2[:], 0.0), writes=[S2])
        vfs = [P.tile([128, 512], F32) for i in range(2)]
        junk = P.tile([128, 512], BF16)
        vbs = [P.tile([128, 4, 512], BF16, "o%d" % i) for i in range(2)]

        def epi(s, nbi, ti, sub, itile, pt, nbw):
            vf, vb = vfs[sub % 2], vbs[s % 2]
            P.add("act", lambda e: e.activation(out=vf[:, :nbw], in_=pt[:, :nbw], func=AF.Gelu, accum_out=S1[:, itile, nbi:nbi + 1]),
                  reads=[pt], writes=[vf, S1])
            P.add("act", lambda e: e.activation(out=junk[:, :nbw], in_=vf[:, :nbw], func=AF.Square, accum_out=S2[:, itile, nbi:nbi + 1]),
                  reads=[vf], writes=[junk, S2])
            P.add("pool", lambda e: e.tensor_copy(out=vb[:, sub, :nbw], in_=vf[:, :nbw]), reads=[vf], writes=[vb])

        def post(s, nbi, ti, t0, tw, nbw):
            vb = vbs[s % 2]
            P.dma("sp", vtok[t0:t0 + tw, nbi * nbw:(nbi + 1) * nbw].rearrange("(a p) n -> p a n", p=128), vb[:, :tw // 128, :nbw],
                  reads=[vb], key=vb.dkey)

        gemm_tm(g_win[j, :, D:2 * D], hT, DC, D, g_bin[j:j + 1, D:2 * D], epi, post)

    def stage_gmlp_spatial(j):
        P.stage()
        H = c.HEADS
        CPH = DC // H
        lng = P.tile([128, DC], F32, "c0")
        lnb = P.tile([128, DC], F32, "c1")
        P.dma("sp", lng[:], g_lngT[j], writes=[lng], key="c0")
        P.dma("sp", lnb[:], g_lnbT[j], writes=[lnb], key="c1")
        bsb = P.tile([128, H, 128], F32, "b0")
        P.dma("sp", bsb[:], g_bs[j].partition_broadcast(128), writes=[bsb], key="b0")
        WcT = P.tile([128, H, 128], BF16)
        Et = P.tile([128, DC, 128], F32)
        wls = [P.tile([128, 128], F32, "r%d" % i) for i in range(2)]
        wms = [P.tile([128, 128], BF16) for i in range(2)]
        for h in range(H):
            wl, wm = wls[h % 2], wms[h % 2]
            pt = ps[4 + h % 2]
            ptb = pt[:].bitcast(BF16)
            P.dma("sp", wl[:], g_ws[j, h], writes=[wl], key=wl.dkey)
            P.add("dve", lambda e, wl=wl, wm=wm: e.tensor_tensor(out=wm[:], in0=wl[:], in1=tril[:], op=ALU.mult), reads=[wl, tril], writes=[wm])
            P.add("pe", lambda e, wm=wm, ptb=ptb: e.transpose(out=ptb[:, 0:128], in_=wm[:], identity=ident[:]), reads=[wm, ident], writes=[pt])
            P.add("act", lambda e, h=h, ptb=ptb: e.copy(out=WcT[:, h, :], in_=ptb[:, 0:128]), reads=[pt], writes=[WcT])
        for h in range(H):
            pt = ps[6 + h % 2]
            P.add("pe", lambda e, h=h, pt=pt: e.matmul(pt[:, 0:128], lhsT=ones_b[:], rhs=WcT[:, h, :], start=True, stop=True),
                  reads=[ones_b, WcT], writes=[pt])
            for cc in range(CPH):
                fc = h * CPH + cc
                P.add("dve", lambda e, h=h, fc=fc, pt=pt: e.scalar_tensor_tensor(out=Et[:, fc, :], in0=pt[:, 0:128], scalar=lnb[:, fc:fc + 1],
                                                                              in1=bsb[:, h, :], op0=ALU.mult, op1=ALU.add),
                      reads=[pt, lnb, bsb], writes=[Et])
        ubs = [P.tile([128, DC, 512], BF16, "x%d" % i) for i in range(2)]
        gos = [P.tile([128, DC, 512], BF16, "o%d" % i) for i in range(2)]
        vts = [P.tile([128, D], BF16, "h%d" % i) for i in range(2)]
        vns = [P.tile([128, D], BF16) for i in range(2)]
        sts = [P.tile([128, 8], F32) for i in range(2)]
        tmps = [P.tile([128, 128], F32) for i in range(4)]

        def load(ti):
            t0, tw = c.ttiles[ti]
            ub = ubs[ti % 2]
            P.dma("sp", ub[:, :, :tw], uT[:, t0:t0 + tw].rearrange("(kc p) t -> p kc t", p=128), writes=[ub], key=ub.dkey)

        def compute(ti):
            t0, tw = c.ttiles[ti]
            ub, go = ubs[ti % 2], gos[ti % 2]
            for sub in range(tw // 128):
                i = t0 // 128 + sub
                vt, vn, st = vts[i % 2], vns[i % 2], sts[i % 2]
                P.dma("sp", vt[:], vtok[i * 128:(i + 1) * 128, :], writes=[vt], key=vt.dkey)
                P.add("dve", lambda e, i=i, st=st: e.tensor_reduce(out=st[:, 0:1], in_=S1[:, i, :], axis=AX.X, op=ALU.add), reads=[S1], writes=[st])
                P.add("dve", lambda e, i=i, st=st: e.tensor_reduce(out=st[:, 1:2], in_=S2[:, i, :], axis=AX.X, op=ALU.add), reads=[S2], writes=[st])
                P.add("dve", lambda e, st=st: e.tensor_scalar(out=st[:, 2:3], in0=st[:, 0:1], scalar1=1.0 / D, scalar2=None, op0=ALU.mult), reads=[st], writes=[st])
                P.add("dve", lambda e, st=st: e.tensor_tensor(out=st[:, 3:4], in0=st[:, 2:3], in1=st[:, 2:3], op=ALU.mult), reads=[st], writes=[st])
                P.add("dve", lambda e, st=st: e.scalar_tensor_tensor(out=st[:, 4:5], in0=st[:, 1:2], scalar=1.0 / D, in1=st[:, 3:4], op0=ALU.mult, op1=ALU.subtract),
                      reads=[st], writes=[st])
                P.add("act", lambda e, st=st: e.activation(out=st[:, 5:6], in_=st[:, 4:5], func=AF.Sqrt, bias=1e-5, scale=1.0), reads=[st], writes=[st])
                P.add("dve", lambda e, st=st: e.reciprocal(out=st[:, 6:7], in_=st[:, 5:6]), reads=[st], writes=[st])
                P.add("dve", lambda e, st=st, vt=vt, vn=vn: e.tensor_scalar(out=vn[:], in0=vt[:], scalar1=st[:, 2:3], scalar2=st[:, 6:7], op0=ALU.subtract, op1=ALU.mult),
                      reads=[vt, st], writes=[vn])
                for fc in range(DC):
                    h = fc // CPH
                    pt = ps[(fc // 4) % 4]
                    tm = tmps[fc % 4]
                    P.add("pe", lambda e, fc=fc, h=h, pt=pt, vn=vn: e.matmul(pt[:, (fc % 4) * 128:(fc % 4 + 1) * 128], lhsT=vn[:, fc * 128:(fc + 1) * 128], rhs=WcT[:, h, :],
                                                                            start=True, stop=True),
                          reads=[vn, WcT], writes=[pt])
                    P.add("dve", lambda e, fc=fc, pt=pt, tm=tm: e.scalar_tensor_tensor(out=tm[:], in0=pt[:, (fc % 4) * 128:(fc % 4 + 1) * 128], scalar=lng[:, fc:fc + 1],
                                                                                    in1=Et[:, fc, :], op0=ALU.mult, op1=ALU.add),
                          reads=[pt, lng, Et], writes=[tm])
                    P.add("pool", lambda e, fc=fc, tm=tm, sub=sub: e.tensor_tensor(out=go[:, fc, sub * 128:(sub + 1) * 128], in0=tm[:], in1=ub[:, fc, sub * 128:(sub + 1) * 128], op=ALU.mult),
                          reads=[tm, ub], writes=[go])
            P.dma("sp", mT[:, t0:t0 + tw].rearrange("(c p) t -> p c t", p=128), go[:, :, :tw], reads=[go], key=go.dkey)

        pipeline(len(c.ttiles), load, compute)

    def stage_zero_slots():
        zi = P.tile([128, NSLOT // 128 * 2], I32)
        P.add("pool", lambda e: e.memset(zi[:], 0), writes=[zi])
        P.dma("sp", slot_tok.rearrange("(p f) o -> p (f o)", p=128), zi[:], reads=[zi], key="k_zs")

    def stage_experts(l):
        P.stage()

        wus = [P.tile([128, UPR], BF16, "w%d" % i) for i in range(2)]
        wdt = P.tile([128, DNR], BF16, "w2")
        sidxs = [P.tile([128, BS], I32, "i%d" % i) for i in range(2)]
        bups = [P.tile([128, 2 * FC], F32, "c%d" % i) for i in range(2)]
        bdr1 = P.tile([128, D], BF16, "b0")
        bdrs = [bdr1, bdr1]
        hgs = [P.tile([128, D], BF16, "h%d" % i) for i in range(2)]
        hbT = P.tile([128, DC, BLK], BF16)
        actT = P.tile([128, FC, BLK], BF16)
        yrs = [P.tile([128, D], BF16, "o%d" % i) for i in range(2)]
        g32, s32, l32 = (P.tile([128, BLK], F32) for _ in range(3))
        wup_v = m_wup.rearrange("l r (a b) -> (l r a) b", b=su)
        wdn_v = m_wdn.rearrange("l r (a b) -> (l r a) b", b=sd)
        bdn_v = m_bdn.rearrange("l r (a b) -> (l r a) b", b=sb)
        bup_v = m_bupT.rearrange("l r f -> (l r) f")
        wu_res = lambda wu: [(wu, a) for a in range(nsu)]
        wd_res = [(wdt, a) for a in range(nsd)]
        bd_res = lambda bdr: [(bdr, a) for a in range(nsb)]

        def load_up(b):
            wu, bup, bdr, sidx = wus[b % 2], bups[b % 2], bdrs[b % 2], sidxs[b % 2]
            P.dma("sp", sidx[:], slot_tok[b * BLK:(b + 1) * BLK, 0:1].rearrange("(s p) o -> p (s o)", p=128),
                  writes=[sidx], key=sidx.dkey, allow_slow_non_contiguous=True)
            for a in range(nsu):
                P.add("pool", lambda e, a=a: e.indirect_dma_start(
                    out=wu[:, a * su:(a + 1) * su], out_offset=None, in_=wup_v,
                    in_offset=bass.IndirectOffsetOnAxis(ap=WIU[:, b, a:a + 1], axis=0), bounds_check=P.reg(e, L * E * 128 * nsu - 1), oob_is_err=False),
                    reads=[WIU], writes=[(wu, a)], dma="%s_s%d" % (wu.dkey, a))
            P.add("pool", lambda e: e.indirect_dma_start(
                out=bup[:], out_offset=None, in_=bup_v,
                in_offset=bass.IndirectOffsetOnAxis(ap=WIDX[:, b:b + 1], axis=0), bounds_check=P.reg(e, L * E * 128 - 1), oob_is_err=False),
                reads=[WIDX], writes=[bup], dma=bup.dkey)

        def load_dn(b):
            bdr = bdr1
            for a in range(nsb):
                P.add("pool", lambda e, a=a: e.indirect_dma_start(
                    out=bdr[:, a * sb:(a + 1) * sb], out_offset=None, in_=bdn_v,
                    in_offset=bass.IndirectOffsetOnAxis(ap=BID[:, b, a:a + 1], axis=0), bounds_check=P.reg(e, L * E * nsb - 1), oob_is_err=False),
                    reads=[BID], writes=[(bdr, a)], dma="%s_s%d" % (bdr.dkey, a))
            for a in range(nsd):
                P.add("pool", lambda e, a=a: e.indirect_dma_start(
                    out=wdt[:, a * sd:(a + 1) * sd], out_offset=None, in_=wdn_v,
                    in_offset=bass.IndirectOffsetOnAxis(ap=WID[:, b, a:a + 1], axis=0), bounds_check=P.reg(e, L * E * 128 * nsd - 1), oob_is_err=False),
                    reads=[WID], writes=[(wdt, a)], dma="w2_s%d" % a)

        gcount = [0]
        ycount = [0]

        def compute(b):
            wu, bup, bdr, sidx = wus[b % 2], bups[b % 2], bdrs[b % 2], sidxs[b % 2]
            wuv = wu[:].rearrange("p (kc f) -> p kc f", kc=DC)
            wdv = wdt[:].rearrange("p (fc d) -> p fc d", fc=FC)
            for jj in range(BS):
                hg = hgs[gcount[0] % 2]
                gcount[0] += 1
                P.add("pool", lambda e, jj=jj, hg=hg: e.indirect_dma_start(
                    out=hg[:], out_offset=None, in_=hrows[:, :],
                    in_offset=bass.IndirectOffsetOnAxis(ap=sidx[:, jj:jj + 1], axis=0), bounds_check=P.reg(e, TC - 1), oob_is_err=False),
                    reads=[sidx], writes=[hg], dma=hg.dkey)
                for c8 in range(0, DC, 8):
                    pt = ps[4 + (c8 // 8) % 2]
                    ptb = pt[:].bitcast(BF16)
                    for cc in range(8):
                        ch = c8 + cc
                        P.add("pe", lambda e, ch=ch, cc=cc, ptb=ptb, hg=hg: e.transpose(out=ptb[:, cc * 128:(cc + 1) * 128], in_=hg[:, ch * 128:(ch + 1) * 128], identity=ident[:]),
                              reads=[hg, ident], writes=[pt])
                    src_v = ptb.rearrange("p (a b) -> p a b", a=8)
                    dst_v = hbT[:, c8:c8 + 8, jj * 128:(jj + 1) * 128]
                    if (c8 // 8) % 2 == 0:
                        P.add("act", lambda e, src_v=src_v, dst_v=dst_v: e.copy(out=dst_v, in_=src_v), reads=[pt], writes=[hbT])
                    else:
                        P.add("dve", lambda e, src_v=src_v, dst_v=dst_v: e.tensor_copy(out=dst_v, in_=src_v), reads=[pt], writes=[hbT])
            for fp in range(FC):
                pg, pl = ps[(2 * fp) % 4], ps[(2 * fp + 1) % 4]
                for (off, pt_) in ((0, pg), (FE, pl)):
                    for kc in range(DC):
                        P.add("pe", lambda e, kc=kc, off=off, pt_=pt_, fp=fp: e.matmul(pt_[:, :BLK], lhsT=wuv[:, kc, off + fp * 128:off + (fp + 1) * 128], rhs=hbT[:, kc, :],
                                                                                   start=(kc == 0), stop=(kc == DC - 1)),
                              reads=wu_res(wu) + [hbT], writes=[pt_])
                P.add("dve", lambda e, fp=fp, pg=pg: e.tensor_scalar(out=g32[:], in0=pg[:, :BLK], scalar1=bup[:, fp:fp + 1], scalar2=7.0, op0=ALU.add, op1=ALU.min),
                      reads=[pg, bup], writes=[g32])
                P.add("act", lambda e: e.activation(out=s32[:], in_=g32[:], func=AF.Sigmoid, scale=1.702), reads=[g32], writes=[s32])
                P.add("dve", lambda e, fp=fp, pl=pl: e.tensor_scalar(out=l32[:], in0=pl[:, :BLK], scalar1=bup[:, FC + fp:FC + fp + 1], scalar2=7.0, op0=ALU.add, op1=ALU.min),
                      reads=[pl, bup], writes=[l32])
                P.add("pool", lambda e: e.tensor_scalar(out=l32[:], in0=l32[:], scalar1=-7.0, scalar2=1.0, op0=ALU.max, op1=ALU.add), reads=[l32], writes=[l32])
                P.add("dve", lambda e: e.tensor_tensor(out=g32[:], in0=g32[:], in1=s32[:], op=ALU.mult), reads=[g32, s32], writes=[g32])
                P.add("dve", lambda e, fp=fp: e.tensor_tensor(out=actT[:, fp, :], in0=g32[:], in1=l32[:], op=ALU.mult), reads=[g32, l32], writes=[actT])
            for jj in range(BS):
                yr = yrs[ycount[0] % 2]
                ycount[0] += 1
                for nb in range(D // NBW):
                    pt = ps[nb % 4]
                    for kf in range(FC):
                        P.add("pe", lambda e, kf=kf, jj=jj, nb=nb, pt=pt: e.matmul(pt[:, :NBW], lhsT=actT[:, kf, jj * 128:(jj + 1) * 128], rhs=wdv[:, kf, nb * NBW:(nb + 1) * NBW],
                                                                                start=(kf == 0), stop=False),
                              reads=[actT] + wd_res, writes=[pt])
                    P.add("pe", lambda e, nb=nb, pt=pt: e.matmul(pt[:, :NBW], lhsT=ones_b[0:1, :], rhs=bdr[0:1, nb * NBW:(nb + 1) * NBW], start=False, stop=True),
                          reads=[ones_b] + bd_res(bdr), writes=[pt])
                    if nb % 2 == 0:
                        P.add("act", lambda e, nb=nb, pt=pt, yr=yr: e.copy(out=yr[:, nb * NBW:(nb + 1) * NBW], in_=pt[:, :NBW]), reads=[pt], writes=[yr])
                    else:
                        P.add("dve", lambda e, nb=nb, pt=pt, yr=yr: e.tensor_copy(out=yr[:, nb * NBW:(nb + 1) * NBW], in_=pt[:, :NBW]), reads=[pt], writes=[yr])
                r0 = b * BLK + jj * 128
                P.dma("sp", ypairs[r0:r0 + 128, :], yr[:], reads=[yr], key=yr.dkey)

        load_up(0)
        load_dn(0)
        if NB > 1:
            load_up(1)
        for b in range(NB):
            compute(b)
            if b + 1 < NB:
                load_dn(b + 1)
            if b + 2 < NB:
                load_up(b + 2)

    def stage_combine(l, last):
        P.stage()
        MG = P.tile([128, D], F32)
        mark = P.sp_off
        emit_mod(l, 1, [(2, MG)])
        P.stage(keep=mark)
        if last:
            gf = P.tile([128, D], F32, "m_g")
            P.dma("sp", gf[:], g_fin.partition_broadcast(128), writes=[gf], key="m_g")
        xts = [P.tile([128, D], F32, "x%d" % i) for i in range(2)]
        yk1 = [P.tile([128, D], BF16, "y0%d" % k) for k in range(4)]
        yks = [yk1, yk1]
        acc = P.tile([128, D], F32)
        acc2 = acc
        xo = xts
        junk = P.tile([128, D], BF16)
        sss = [P.tile([128, 4], F32) for i in range(2)]

        def load(i):
            P.dma("sp", xts[i % 2][:], xres[i * 128:(i + 1) * 128, :], writes=[xts[i % 2]], key=xts[i % 2].dkey)

        def gathers(i):
            for k in range(4):
                yk = yk1[k]
                P.add("pool", lambda e, i=i, k=k, yk=yk: e.indirect_dma_start(
                    out=yk[:], out_offset=None, in_=ypairs[:, :],
                    in_offset=bass.IndirectOffsetOnAxis(ap=IDX[:, i, k:k + 1], axis=0), bounds_check=P.reg(e, NSLOT + 127), oob_is_err=False),
                    reads=[IDX], writes=[yk], dma=yk.dkey)

        def compute(i):
            xt, ys, o, ss = xts[i % 2], yks[i % 2], xo[i % 2], sss[i % 2]
            P.add("dve", lambda e: e.tensor_scalar(out=acc[:], in0=ys[0][:], scalar1=GATE[:, i, 0:1], scalar2=None, op0=ALU.mult),
                  reads=[ys[0], GATE], writes=[acc])
            for k in range(1, 4):
                P.add("dve", lambda e, k=k: e.scalar_tensor_tensor(out=acc[:], in0=ys[k][:], scalar=GATE[:, i, k:k + 1], in1=acc[:], op0=ALU.mult, op1=ALU.add),
                      reads=[ys[k], GATE, acc], writes=[acc])
            if i + 1 < NT:
                gathers(i + 1)
            P.add("dve", lambda e: e.tensor_tensor(out=acc[:], in0=acc[:], in1=MG[:], op=ALU.mult), reads=[acc, MG], writes=[acc])
            P.add("pool", lambda e: e.tensor_tensor(out=o[:], in0=acc[:], in1=xt[:], op=ALU.add), reads=[acc, xt], writes=[o])
            if not last:
                P.dma("sp", xres[i * 128:(i + 1) * 128, :], o[:], reads=[o], key=o.dkey)
            else:
                if debug:
                    P.dma("sp", xres[i * 128:(i + 1) * 128, :], o[:], reads=[o], key=o.dkey)
                if i * 128 >= c.HALO:
                    P.add("act", lambda e: e.activation(out=junk[:], in_=o[:], func=AF.Square, accum_out=ss[:, 0:1]), reads=[o], writes=[junk, ss])
                    P.add("act", lambda e: e.activation(out=ss[:, 1:2], in_=ss[:, 0:1], func=AF.Sqrt, bias=1e-5, scale=1.0 / D), reads=[ss], writes=[ss])
                    P.add("dve", lambda e: e.reciprocal(out=ss[:, 2:3], in_=ss[:, 1:2]), reads=[ss], writes=[ss])
                    P.add("dve", lambda e: e.scalar_tensor_tensor(out=acc2[:], in0=o[:], scalar=ss[:, 2:3], in1=gf[:], op0=ALU.mult, op1=ALU.mult),
                          reads=[o, ss, gf], writes=[acc2])
                    r0 = i * 128 - c.HALO
                    P.dma("sp", out[r0:r0 + 128, :], acc2[:], reads=[acc2], key="k_out")

        gathers(0)
        pipeline(NT, load, compute)

    stages = []
    stages.append(("const", stage_const))
    for l in range(L):
        j = l // 2
        src = x_in if l == 0 else xres
        stages.append(("norm1_%d" % l, lambda l=l, src=src: stage_norm(l, 0, src, False)))
        if l % 2 == 0:
            stages.append(("pool_in_%d" % l, lambda j=j: stage_pool_in(j)))
            stages.append(("pool_grp_%d" % l, lambda j=j: stage_pool_group(j)))
            stages.append(("pool_out_%d" % l, lambda l=l, j=j, src=src: (stage_out_gemm(l, p_wout[j], src), stage_zero_slots())))
        else:
            stages.append(("gmlp_u_%d" % l, lambda j=j: stage_gmlp_u(j)))
            stages.append(("gmlp_v_%d" % l, lambda j=j: stage_gmlp_v(j)))
            stages.append(("gmlp_sp_%d" % l, lambda j=j: stage_gmlp_spatial(j)))
            stages.append(("gmlp_out_%d" % l, lambda l=l, j=j, src=src: (stage_out_gemm(l, g_wout[j], src), stage_zero_slots())))
        stages.append(("norm2_%d" % l, lambda l=l: stage_norm(l, 1, xres, True)))
        stages.append(("experts_%d" % l, lambda l=l: stage_experts(l)))
        stages.append(("combine_%d" % l, lambda l=l: stage_combine(l, l == L - 1)))
    P.usage = []
    for si, (name, fn) in enumerate(stages):
        if stop_after is not None and si > stop_after:
            break
        P.hwm = 0
        fn()
        P.usage.append((name, P.hwm))
    if not dry:
        P.emit()
    P.stage_names = [n for n, _ in stages]
    P.dbg = dict(WIU=WIU, WID=WID, BID=BID, WIDX=WIDX)
    return nc, P, stack


def _colT(v):
    v = np.asarray(v)
    sh = v.shape
    return np.ascontiguousarray(v.reshape(sh[:-1] + (sh[-1] // 128, 128)).swapaxes(-1, -2))


def make_in_maps(cfg, inp):
    c = cfg
    f32 = np.float32
    x = np.asarray(inp["x"], dtype=f32)
    cvec = np.asarray(inp["c"], dtype=f32)
    shared = {
        "ident": np.eye(128, dtype=f32),
        "triu": np.triu(np.ones((128, 128), f32), 1),
        "tril": np.tril(np.ones((128, 128), f32), 0),
        "thr": (np.arange(c.NB, dtype=f32) * c.BLK)[None, :],
        "pcol": np.arange(128, dtype=f32)[:, None],
        "tokid": np.repeat((np.arange(c.NT, dtype=np.int32)[None, :] * 128 + np.arange(128, dtype=np.int32)[:, None]), 2, axis=1).astype(np.int32),
        "w_cond": np.asarray(inp["w_cond"], f32),
        "b_condT": _colT(np.asarray(inp["b_cond"], f32)),
        "w_mod": np.asarray(inp["w_mod"], f32),
        "b_mod": np.asarray(inp["b_mod"], f32),
        "g_norm_mix": np.asarray(inp["g_norm_mix"], f32),
        "g_norm_ffn": np.asarray(inp["g_norm_ffn"], f32),
        "pool_w_in": np.asarray(inp["pool_w_in"], f32),
        "pool_w_group": np.asarray(inp["pool_w_group"], f32),
        "pool_scaleT": _colT(np.asarray(inp["pool_scale"], f32)),
        "pool_w_out": np.asarray(inp["pool_w_out"], f32),
        "gmlp_w_in": np.asarray(inp["gmlp_w_in"], f32),
        "gmlp_b_inT": _colT(np.asarray(inp["gmlp_b_in"], f32)[:, :c.D]),
        "gmlp_b_in": np.asarray(inp["gmlp_b_in"], f32),
        "gmlp_ln_gT": _colT(np.asarray(inp["gmlp_ln_g"], f32)),
        "gmlp_ln_bT": _colT(np.asarray(inp["gmlp_ln_b"], f32)),
        "gmlp_w_s": np.asarray(inp["gmlp_w_s"], f32),
        "gmlp_b_s": np.asarray(inp["gmlp_b_s"], f32),
        "gmlp_w_out": np.asarray(inp["gmlp_w_out"], f32),
        "moe_w_router": np.asarray(inp["moe_w_router"], f32),
        "moe_b_router": np.asarray(inp["moe_b_router"], f32),
        "moe_w_upP": np.ascontiguousarray(np.asarray(inp["moe_w_up"], f32).reshape(c.DEPTH, c.E, c.DC, 128, 2 * c.FE).transpose(0, 1, 3, 2, 4)).reshape(c.DEPTH, c.E * 128, c.DC * 2 * c.FE),
        "moe_b_upT": _colT(np.asarray(inp["moe_b_up"], f32)).reshape(c.DEPTH, c.E * 128, 2 * c.FC),
        "moe_w_downP": np.ascontiguousarray(np.asarray(inp["moe_w_down"], f32).reshape(c.DEPTH, c.E, c.FC, 128, c.D).transpose(0, 1, 3, 2, 4)).reshape(c.DEPTH, c.E * 128, c.FC * c.D),
        "moe_b_down": np.asarray(inp["moe_b_down"], f32),
        "g_final": np.asarray(inp["g_final"], f32)[None, :],
    }
    maps = []
    wins = (2, 4, 8, 16)
    for core in range(c.BATCH * c.NQ):
        b, q = core // c.NQ, core % c.NQ
        start = q * c.SQ - c.HALO
        xc = np.zeros((c.TC, c.D), f32)
        lo = max(start, 0)
        xc[lo - start:, :] = x[b, lo:start + c.TC, :]
        pos = start + np.arange(c.TC)
        tokc = np.ones((5, c.TC), f32)
        tokc[0] = (pos >= 0).astype(f32)
        for gi, w in enumerate(wins):
            cnt = np.minimum(np.maximum(pos, 0) + 1, w).astype(f32)
            tokc[1 + gi] = 1.0 / cnt
        m = dict(shared)
        m["x"] = xc
        m["cT"] = _colT(cvec[b])
        m["tokc"] = tokc
        m["validT"] = np.ascontiguousarray(tokc[0].reshape(c.NT, 128).T)
        maps.append(m)
    return maps


_CACHE = {}


def kernel(**inputs):
    cfg = Cfg()
    if "nc" not in _CACHE:
        _CACHE["nc"] = build(cfg)
    nc, P, stack = _CACHE["nc"]
    maps = make_in_maps(cfg, inputs)
    ncores = cfg.BATCH * cfg.NQ
    res = run_bass_kernel_spmd(nc, maps, core_ids=list(range(ncores)))
    outp = np.empty((cfg.BATCH, cfg.SEQ, cfg.D), np.float32)
    for core in range(ncores):
        b, q = core // cfg.NQ, core % cfg.NQ
        outp[b, q * cfg.SQ:(q + 1) * cfg.SQ, :] = res.results[core]["out"]
    return outp
```
